# Optimizing a Trainium2 kernel written in Bass

```python
import jax, jax.numpy as jnp
from jax import lax
import numpy as np

D_MODEL = 2048
BATCH = 4
SEQ = 2048
DEPTH = 1

NSA_HEAD_DIM = 64
NSA_WIDTH = D_MODEL // 2
N_NSA_HEADS = NSA_WIDTH // NSA_HEAD_DIM
N_KV_HEADS = N_NSA_HEADS // 4
GQA_GROUP = N_NSA_HEADS // N_KV_HEADS
KV_WIDTH = N_KV_HEADS * NSA_HEAD_DIM
N_BRANCHES = 3
GMLP_WIDTH = D_MODEL - NSA_WIDTH
GMLP_GROUP_DIM = 128
N_GMLP_GROUPS = GMLP_WIDTH // GMLP_GROUP_DIM
GMLP_CHUNK = 128
CMP_BLOCK = 32
CMP_STRIDE = 16
CMP_HIDDEN = 256
SLC_BLOCK = 64
SLC_TOPK = 16
WINDOW = 512
Q_BLOCK = 64
FORCE_BONUS = 100.0
ROPE_THETA = 10000.0
N_EXPERTS = 64
N_EXPERT_GROUPS = 8
TOPK_GROUPS = 4
TOPK_EXPERTS = 8
EXPERT_DIM = 512
SHARED_DIM = 512
ROUTED_SCALE = 2.5
MOE_BLOCK = 128
EPS = 1e-6
NEG = -1e30
IN_COLS = NSA_WIDTH + 6 * KV_WIDTH + N_NSA_HEADS * N_BRANCHES + 2 * GMLP_WIDTH

kernel_name = "hymba_nsa_gmlp_moe_adaln_layer"

F32 = jnp.float32


def rms_norm(x, g):
    xf = x.astype(F32)
    y = xf * lax.rsqrt(jnp.mean(xf * xf, axis=-1, keepdims=True) + EPS)
    return (y * g.astype(F32)).astype(x.dtype)


def rope(x, pos):
    half = x.shape[-1] // 2
    freqs = ROPE_THETA ** (-jnp.arange(half, dtype=F32) / half)
    ang = pos[:, None] * freqs[None, :]
    cos, sin = jnp.cos(ang), jnp.sin(ang)
    xf = x.astype(F32)
    x1, x2 = xf[..., :half], xf[..., half:]
    return jnp.concatenate([x1 * cos - x2 * sin, x2 * cos + x1 * sin], axis=-1).astype(x.dtype)


def masked_softmax(s, mask):
    p = jax.nn.softmax(jnp.where(mask, s, NEG), axis=-1)
    return p * mask


def compress(kv, pos_emb, w1, w2):
    T = kv.shape[2]
    n_cmp = (T - CMP_BLOCK) // CMP_STRIDE + 1
    idx = jnp.arange(n_cmp)[:, None] * CMP_STRIDE + jnp.arange(CMP_BLOCK)[None, :]
    blocks = kv[:, :, idx, :] + pos_emb
    flat = blocks.reshape(blocks.shape[0], blocks.shape[1], n_cmp, CMP_BLOCK * NSA_HEAD_DIM)
    return jax.nn.gelu(flat @ w1) @ w2


def nsa(q, k_c, v_c, k_s, v_s, k_w, v_w, gate, g_q, g_k,
        cmp_pos_k, cmp_pos_v, cmp_w1_k, cmp_w2_k, cmp_w1_v, cmp_w2_v):
    B, H, T, hd = q.shape
    scale = hd ** -0.5
    pos = jnp.arange(T, dtype=F32)
    t_idx = jnp.arange(T)
    q = rope(rms_norm(q, g_q), pos)
    k_s = rope(rms_norm(k_s, g_k), pos)
    k_w = rope(rms_norm(k_w, g_k), pos)
    qf = q.reshape(B, N_KV_HEADS, GQA_GROUP, T, hd).astype(F32) * scale

    n_cmp = (T - CMP_BLOCK) // CMP_STRIDE + 1
    cmp_start = jnp.arange(n_cmp) * CMP_STRIDE
    kc = compress(k_c, cmp_pos_k, cmp_w1_k, cmp_w2_k)
    vc = compress(v_c, cmp_pos_v, cmp_w1_v, cmp_w2_v)
    kc = rope(rms_norm(kc, g_k), cmp_start.astype(F32) + (CMP_BLOCK - 1) / 2)
    cmp_mask = (cmp_start + CMP_BLOCK - 1)[None, :] <= t_idx[:, None]
    s_c = jnp.einsum('bhgtd,bhnd->bhgtn', qf, kc.astype(F32))
    p_c = masked_softmax(s_c, cmp_mask)
    o_c = jnp.einsum('bhgtn,bhnd->bhgtd', p_c.astype(vc.dtype), vc)

    n_slc = T // SLC_BLOCK
    slc_start = jnp.arange(n_slc) * SLC_BLOCK
    overlap = ((cmp_start[:, None] <= slc_start[None, :] + SLC_BLOCK - 1)
               & (cmp_start[:, None] + CMP_BLOCK - 1 >= slc_start[None, :])).astype(F32)
    imp = jnp.einsum('bhgtn,nj->bhtj', p_c, overlap)
    cur = t_idx // SLC_BLOCK
    j = jnp.arange(n_slc)
    visible = j[None, :] <= cur[:, None]
    forced = (j[None, :] == 0) | (j[None, :] == cur[:, None]) | (j[None, :] == cur[:, None] - 1)
    score = jnp.where(visible, imp + jnp.where(forced, FORCE_BONUS, 0.0), -1.0)
    k_sel = min(SLC_TOPK, n_slc)
    top_val, top_idx = lax.top_k(score, k_sel)
    top_ok = top_val >= 0.0

    ks_blocks = k_s.reshape(B, N_KV_HEADS, n_slc, SLC_BLOCK, hd)
    vs_blocks = v_s.reshape(B, N_KV_HEADS, n_slc, SLC_BLOCK, hd)
    kw_pad = jnp.pad(k_w, ((0, 0), (0, 0), (WINDOW, 0), (0, 0)))
    vw_pad = jnp.pad(v_w, ((0, 0), (0, 0), (WINDOW, 0), (0, 0)))
    b_ix = jnp.arange(B)[:, None, None, None]
    h_ix = jnp.arange(N_KV_HEADS)[None, :, None, None]
    n_sel_keys = k_sel * SLC_BLOCK

    def query_block(i):
        t0 = i * Q_BLOCK
        tq = t0 + jnp.arange(Q_BLOCK)
        q_b = lax.dynamic_slice_in_dim(qf, t0, Q_BLOCK, axis=3)
        idx = lax.dynamic_slice_in_dim(top_idx, t0, Q_BLOCK, axis=2)
        ok = lax.dynamic_slice_in_dim(top_ok, t0, Q_BLOCK, axis=2)
        k_g = ks_blocks[b_ix, h_ix, idx].reshape(B, N_KV_HEADS, Q_BLOCK, n_sel_keys, hd)
        v_g = vs_blocks[b_ix, h_ix, idx].reshape(B, N_KV_HEADS, Q_BLOCK, n_sel_keys, hd)
        key_pos = idx[..., None] * SLC_BLOCK + jnp.arange(SLC_BLOCK)
        sel_mask = (ok[..., None] & (key_pos <= tq[:, None, None])).reshape(B, N_KV_HEADS, Q_BLOCK, n_sel_keys)
        s_s = jnp.einsum('bhgqd,bhqmd->bhgqm', q_b, k_g.astype(F32))
        p_s = masked_softmax(s_s, sel_mask[:, :, None])
        o_s = jnp.einsum('bhgqm,bhqmd->bhgqd', p_s.astype(v_g.dtype), v_g)
        kw_b = lax.dynamic_slice_in_dim(kw_pad, t0, Q_BLOCK + WINDOW, axis=2)
        vw_b = lax.dynamic_slice_in_dim(vw_pad, t0, Q_BLOCK + WINDOW, axis=2)
        kpos = t0 - WINDOW + jnp.arange(Q_BLOCK + WINDOW)
        win_mask = ((kpos[None, :] >= 0) & (kpos[None, :] <= tq[:, None])
                    & (kpos[None, :] > tq[:, None] - WINDOW))
        s_w = jnp.einsum('bhgqd,bhkd->bhgqk', q_b, kw_b.astype(F32))
        p_w = masked_softmax(s_w, win_mask)
        o_w = jnp.einsum('bhgqk,bhkd->bhgqd', p_w.astype(vw_b.dtype), vw_b)
        return o_s, o_w

    o_s, o_w = lax.map(query_block, jnp.arange(T // Q_BLOCK))

    def unblock(o):
        return jnp.moveaxis(o, 0, 3).reshape(B, N_KV_HEADS, GQA_GROUP, T, hd)

    g = jax.nn.sigmoid(gate.astype(F32)).transpose(0, 2, 1, 3).reshape(B, N_KV_HEADS, GQA_GROUP, T, N_BRANCHES)
    o = g[..., 0:1] * o_c + g[..., 1:2] * unblock(o_s) + g[..., 2:3] * unblock(o_w)
    return o.reshape(B, H, T, hd).transpose(0, 2, 1, 3).reshape(B, T, H * hd).astype(q.dtype)


def chunked_gmlp(u, v, g_v, w_spatial, b_spatial):
    B, T, _ = u.shape
    n_chunk = T // GMLP_CHUNK
    u = jax.nn.gelu(u)
    v = rms_norm(jax.nn.gelu(v).reshape(B, n_chunk, GMLP_CHUNK, N_GMLP_GROUPS, GMLP_GROUP_DIM), g_v)
    causal = jnp.tril(jnp.ones((GMLP_CHUNK, GMLP_CHUNK), dtype=bool))
    w = jnp.where(causal, w_spatial, 0.0)
    mixed = jnp.einsum('gts,bcsgd->bctgd', w, v) + b_spatial.T[None, None, :, :, None]
    return u * mixed.reshape(B, T, GMLP_WIDTH)


def moe(h, w_router, router_bias, w_gate, w_up, w_down, ws_gate, ws_up, ws_down):
    N, D = h.shape
    s = jax.nn.sigmoid(h.astype(F32) @ w_router.astype(F32))
    sb = s + router_bias.astype(F32)
    grp_score = lax.top_k(sb.reshape(N, N_EXPERT_GROUPS, N_EXPERTS // N_EXPERT_GROUPS), 2)[0].sum(-1)
    _, gidx = lax.top_k(grp_score, TOPK_GROUPS)
    gmask = jnp.any(gidx[..., None] == jnp.arange(N_EXPERT_GROUPS), axis=-2)
    emask = jnp.repeat(gmask, N_EXPERTS // N_EXPERT_GROUPS, axis=-1)
    _, eidx = lax.top_k(jnp.where(emask, sb, NEG), TOPK_EXPERTS)
    w = jnp.take_along_axis(s, eidx, axis=-1)
    w = w / jnp.sum(w, axis=-1, keepdims=True) * ROUTED_SCALE

    nk = N * TOPK_EXPERTS
    flat_e = eidx.reshape(-1).astype(jnp.int32)
    order = jnp.argsort(flat_e, stable=True)
    e_sorted = flat_e[order]
    tok_sorted = (order // TOPK_EXPERTS).astype(jnp.int32)
    w_sorted = w.reshape(-1)[order]
    counts = jnp.zeros(N_EXPERTS, jnp.int32).at[flat_e].add(1)
    padded = (counts + MOE_BLOCK - 1) // MOE_BLOCK * MOE_BLOCK
    start = jnp.cumsum(counts) - counts
    pend = jnp.cumsum(padded)
    pstart = pend - padded
    dest = pstart[e_sorted] + jnp.arange(nk, dtype=jnp.int32) - start[e_sorted]
    cap = nk + N_EXPERTS * MOE_BLOCK
    n_blk = cap // MOE_BLOCK
    buf_tok = jnp.zeros(cap, jnp.int32).at[dest].set(tok_sorted)
    buf_w = jnp.zeros(cap, F32).at[dest].set(w_sorted)
    blk_e = jnp.minimum(jnp.searchsorted(pend, jnp.arange(n_blk, dtype=jnp.int32) * MOE_BLOCK, side='right'),
                        N_EXPERTS - 1)

    def expert_block(args):
        tok, e = args
        xb = h[tok]
        hid = jax.nn.silu(xb @ w_gate[e]) * (xb @ w_up[e])
        return hid @ w_down[e]

    out = lax.map(expert_block, (buf_tok.reshape(n_blk, MOE_BLOCK), blk_e))
    routed = jax.ops.segment_sum(out.reshape(cap, D) * buf_w[:, None].astype(out.dtype), buf_tok,
                                 num_segments=N)
    shared = (jax.nn.silu(h @ ws_gate) * (h @ ws_up)) @ ws_down
    return routed + shared


def hybrid_layer(x, c, w_ada, b_ada, g_norm1, w_in, g_q, g_k, cmp_pos_k, cmp_pos_v, cmp_w1_k, cmp_w2_k,
                 cmp_w1_v, cmp_w2_v, g_gmlp_v, w_spatial, b_spatial, g_out_nsa, g_out_gmlp, w_out, g_norm2,
                 w_router, router_bias, w_gate, w_up, w_down, ws_gate, ws_up, ws_down):
    B, T, D = x.shape
    mod = jax.nn.silu(c) @ w_ada + b_ada
    sh1, sc1, gt1, sh2, sc2, gt2 = jnp.split(mod[:, None, :], 6, axis=-1)

    h = rms_norm(x, g_norm1) * (1 + sc1) + sh1
    p = h @ w_in
    widths = [NSA_WIDTH] + [KV_WIDTH] * 6 + [N_NSA_HEADS * N_BRANCHES, GMLP_WIDTH]
    q, kc, vc, ks, vs, kw, vw, gate, u, v = jnp.split(p, [int(o) for o in np.cumsum(widths)], axis=-1)

    def heads(t, n):
        return t.reshape(B, T, n, NSA_HEAD_DIM).transpose(0, 2, 1, 3)

    o_nsa = nsa(heads(q, N_NSA_HEADS), heads(kc, N_KV_HEADS), heads(vc, N_KV_HEADS),
                heads(ks, N_KV_HEADS), heads(vs, N_KV_HEADS), heads(kw, N_KV_HEADS), heads(vw, N_KV_HEADS),
                gate.reshape(B, T, N_NSA_HEADS, N_BRANCHES), g_q, g_k,
                cmp_pos_k, cmp_pos_v, cmp_w1_k, cmp_w2_k, cmp_w1_v, cmp_w2_v)
    o_gmlp = chunked_gmlp(u, v, g_gmlp_v, w_spatial, b_spatial)
    mix = jnp.concatenate([rms_norm(o_nsa, g_out_nsa), rms_norm(o_gmlp, g_out_gmlp)], axis=-1) @ w_out
    x = x + gt1 * mix

    h2 = rms_norm(x, g_norm2) * (1 + sc2) + sh2
    y = moe(h2.reshape(B * T, D), w_router, router_bias, w_gate, w_up, w_down, ws_gate, ws_up, ws_down)
    return x + gt2 * y.reshape(B, T, D)


def setup_inputs(seed: int = 0) -> dict:
    key = jax.random.key(seed)
    keys = iter(jax.random.split(key, 32))
    L = DEPTH
    hd = NSA_HEAD_DIM

    def normal(shape, scale):
        return scale * jax.random.normal(next(keys), shape, F32)

    def gain(shape, noise=0.02):
        return 1.0 + noise * jax.random.normal(next(keys), shape, F32)

    return {
        "x": normal((BATCH, SEQ, D_MODEL), 1.0),
        "c": normal((BATCH, D_MODEL), 1.0),
        "w_ada": normal((L, D_MODEL, 6 * D_MODEL), 0.5 * D_MODEL ** -0.5),
        "b_ada": normal((L, 6 * D_MODEL), 0.02),
        "g_norm1": gain((L, D_MODEL)),
        "w_in": normal((L, D_MODEL, IN_COLS), D_MODEL ** -0.5),
        "g_q": gain((L, hd)),
        "g_k": gain((L, hd)),
        "cmp_pos_k": normal((L, CMP_BLOCK, hd), 0.1),
        "cmp_pos_v": normal((L, CMP_BLOCK, hd), 0.1),
        "cmp_w1_k": normal((L, CMP_BLOCK * hd, CMP_HIDDEN), (CMP_BLOCK * hd) ** -0.5),
        "cmp_w2_k": normal((L, CMP_HIDDEN, hd), CMP_HIDDEN ** -0.5),
        "cmp_w1_v": normal((L, CMP_BLOCK * hd, CMP_HIDDEN), (CMP_BLOCK * hd) ** -0.5),
        "cmp_w2_v": normal((L, CMP_HIDDEN, hd), CMP_HIDDEN ** -0.5),
        "g_gmlp_v": gain((L, N_GMLP_GROUPS, GMLP_GROUP_DIM)),
        "w_spatial": normal((L, N_GMLP_GROUPS, GMLP_CHUNK, GMLP_CHUNK), 0.5 * GMLP_CHUNK ** -0.5),
        "b_spatial": gain((L, N_GMLP_GROUPS, GMLP_CHUNK), 0.1),
        "g_out_nsa": gain((L, NSA_WIDTH)),
        "g_out_gmlp": gain((L, GMLP_WIDTH)),
        "w_out": normal((L, D_MODEL, D_MODEL), D_MODEL ** -0.5),
        "g_norm2": gain((L, D_MODEL)),
        "w_router": normal((L, D_MODEL, N_EXPERTS), D_MODEL ** -0.5),
        "router_bias": normal((L, N_EXPERTS), 0.01),
        "w_gate": normal((L, N_EXPERTS, D_MODEL, EXPERT_DIM), D_MODEL ** -0.5),
        "w_up": normal((L, N_EXPERTS, D_MODEL, EXPERT_DIM), D_MODEL ** -0.5),
        "w_down": normal((L, N_EXPERTS, EXPERT_DIM, D_MODEL), EXPERT_DIM ** -0.5),
        "ws_gate": normal((L, D_MODEL, SHARED_DIM), D_MODEL ** -0.5),
        "ws_up": normal((L, D_MODEL, SHARED_DIM), D_MODEL ** -0.5),
        "ws_down": normal((L, SHARED_DIM, D_MODEL), SHARED_DIM ** -0.5),
    }


def reference(x, c, w_ada, b_ada, g_norm1, w_in, g_q, g_k, cmp_pos_k, cmp_pos_v, cmp_w1_k, cmp_w2_k,
              cmp_w1_v, cmp_w2_v, g_gmlp_v, w_spatial, b_spatial, g_out_nsa, g_out_gmlp, w_out, g_norm2,
              w_router, router_bias, w_gate, w_up, w_down, ws_gate, ws_up, ws_down):
    for l in range(DEPTH):
        x = hybrid_layer(x, c, w_ada[l], b_ada[l], g_norm1[l], w_in[l], g_q[l], g_k[l], cmp_pos_k[l],
                         cmp_pos_v[l], cmp_w1_k[l], cmp_w2_k[l], cmp_w1_v[l], cmp_w2_v[l], g_gmlp_v[l],
                         w_spatial[l], b_spatial[l], g_out_nsa[l], g_out_gmlp[l], w_out[l], g_norm2[l],
                         w_router[l], router_bias[l], w_gate[l], w_up[l], w_down[l], ws_gate[l], ws_up[l],
                         ws_down[l])
    return x
```

```python
import os
import numpy as np
import ml_dtypes
from contextlib import ExitStack
import concourse.bass as bass
import concourse.mybir as mybir
from concourse.bass_utils import run_bass_kernel_spmd

F32 = mybir.dt.float32
BF16 = mybir.dt.bfloat16
AF = mybir.ActivationFunctionType
ALU = mybir.AluOpType
AX = mybir.AxisListType

D = 2048
DC = 16
NT = 8
NS = 16
TOK = 1024
SLOTS = 2048
NH = 16
NKV = 4
HD = 64
NE = 64
EPS = 1e-6
BIG = 30000.0
IN_COLS = 4656
GORDER = [0, 2, 1, 3]


class Buf:
    def __init__(self, name):
        self.name = name
        self.w = None
        self.r = []


class Sched:
    def __init__(self, nc, es, n_dma_sems=32):
        self.nc = nc
        self.eng = {"pe": nc.tensor, "act": nc.scalar, "dve": nc.vector, "pool": nc.gpsimd, "sp": nc.sync}
        self.sem = {}
        self.cnt = {}
        for k in self.eng:
            self.sem[k] = es.enter_context(nc.semaphore("s_" + k))
            self.cnt[k] = 0
        self.dsem = [es.enter_context(nc.semaphore("d%d" % i)) for i in range(n_dma_sems)]
        self.dcnt = [0] * n_dma_sems
        self.dnext = 0
        self.known = {k: {} for k in self.eng}
        self.ninst = 0

    def _semobj(self, key):
        return self.sem[key] if isinstance(key, str) else self.dsem[key]

    def _wait(self, e, key, val):
        if key == e and e == "pe":
            return
        kn = self.known[e]
        if kn.get(key, 0) >= val:
            return
        self.eng[e].wait_ge(self._semobj(key), val)
        kn[key] = val

    def _deps(self, e, reads, writes):
        for b in reads:
            if b.w is not None:
                self._wait(e, *b.w)
        for b in writes:
            if b.w is not None:
                self._wait(e, *b.w)
            for r in b.r:
                self._wait(e, *r)

    def _mark(self, tok, reads, writes):
        for b in reads:
            b.r.append(tok)
            if len(b.r) > 24:
                b.r = b.r[-24:] if False else self._compress(b.r)
        for b in writes:
            b.w = tok
            b.r = []

    @staticmethod
    def _compress(toks):
        best = {}
        for k, v in toks:
            if best.get(k, 0) < v:
                best[k] = v
        return list(best.items())

    def op(self, e, fn, reads=(), writes=()):
        self._deps(e, reads, writes)
        inst = fn(self.eng[e])
        self.cnt[e] += 1
        inst.then_inc(self.sem[e], 1)
        self.ninst += 1
        self._mark((e, self.cnt[e]), reads, writes)
        return inst

    def mm(self, fns, reads=(), writes=()):
        e = "pe"
        self._deps(e, reads, writes)
        inst = None
        for fn in fns:
            inst = fn(self.eng[e])
            self.ninst += 1
        self.cnt[e] += 1
        inst.then_inc(self.sem[e], 1)
        self._mark((e, self.cnt[e]), reads, writes)

    def dma(self, e, out, in_, reads=(), writes=(), **kw):
        self._deps(e, reads, writes)
        i = self.dnext
        self.dnext = (self.dnext + 1) % len(self.dsem)
        if self.dcnt[i] > 0:
            self._wait(e, i, self.dcnt[i])
        inst = self.eng[e].dma_start(out=out, in_=in_, **kw)
        self.dcnt[i] += 16
        inst.then_inc(self.dsem[i], 16)
        self.ninst += 1
        self._mark((i, self.dcnt[i]), reads, writes)

    def finish(self, e, bufs):
        for b in bufs:
            if b.w is not None:
                self._wait(e, *b.w)


class Tile:
    def __init__(self, ap, buf, off, nbytes):
        self.ap = ap
        self.buf = buf
        self.off = off
        self.nbytes = nbytes

    def __getitem__(self, idx):
        return self.ap[idx]


class Arena:
    def __init__(self, nc, es, kb=206):
        self.total = kb * 1024
        self.t = es.enter_context(nc.sbuf_tensor("arena", [128, self.total // 4], F32))
        self.used = []
        self.ghosts = []
        self.peak = 0

    def alloc(self, name, shape, dtype, parts=128):
        esz = 4 if dtype == F32 else 2
        n = 1
        for s in shape:
            n *= s
        nbytes = (n * esz + 63) // 64 * 64
        off = 0
        for (o, e_, _) in sorted(self.used, key=lambda u: u[0]):
            if off + nbytes <= o:
                break
            off = max(off, e_)
        if off + nbytes > self.total:
            raise RuntimeError("SBUF arena OOM for %s (%d B); used=%s" % (
                name, nbytes, [(u[2].buf.name, u[1] - u[0]) for u in self.used]))
        ap = self.t[0:parts, off // 4:(off + nbytes) // 4]
        if dtype != F32:
            ap = ap.bitcast(dtype)
        ap = ap[:, 0:n]
        if len(shape) == 2:
            ap = ap.rearrange("p (a b) -> p a b", b=shape[1])
        elif len(shape) == 3:
            ap = ap.rearrange("p (a b c) -> p a b c", b=shape[1], c=shape[2])
        buf = Buf(name)
        toks = []
        keep = []
        for (o, e_, tk) in self.ghosts:
            if o < off + nbytes and off < e_:
                toks.extend(tk)
                if o < off:
                    keep.append((o, off, tk))
                if e_ > off + nbytes:
                    keep.append((off + nbytes, e_, tk))
            else:
                keep.append((o, e_, tk))
        self.ghosts = keep
        buf.r = Sched._compress(toks)
        t = Tile(ap, buf, off, nbytes)
        self.used.append((off, off + nbytes, t))
        self.peak = max(self.peak, max(u[1] for u in self.used))
        return t

    def free(self, *tiles):
        for t in tiles:
            self.used = [u for u in self.used if u[2] is not t]
            toks = list(t.buf.r)
            if t.buf.w is not None:
                toks.append(t.buf.w)
            self.ghosts.append((t.off, t.off + t.nbytes, Sched._compress(toks)))


class Ring:
    def __init__(self, items):
        self.items = items
        self.i = 0

    def next(self):
        it = self.items[self.i]
        self.i = (self.i + 1) % len(self.items)
        return it


def build_program(stop=None, dbg=()):
    nc = bass.Bass("TRN2", target_bir_lowering=False)

    declared = []

    def din(name, shape, dt=F32):
        declared.append(name)
        return nc.dram_tensor(name, list(shape), dt, kind="ExternalInput").ap()

    xkv = din("xkv", [SLOTS, D])
    ct = din("ct", [128, DC])
    w_ada = din("w_ada", [D, 6 * D])
    b_ada = din("b_ada", [1, 6 * D])
    gn1 = din("gn1", [128, DC])
    gn2 = din("gn2", [128, DC])
    gout = din("gout", [128, DC])
    w_in = din("w_in", [D, IN_COLS])
    g_q = din("g_q", [1, HD])
    g_k = din("g_k", [1, HD])
    posk = din("posk", [HD, 32])
    posv = din("posv", [HD, 32])
    w1k = din("w1k", [2048, 256])
    w1v = din("w1v", [2048, 256])
    w2k = din("w2k", [256, HD])
    w2v = din("w2v", [256, HD])
    g_gv = din("g_gv", [1, 1024])
    wspT = din("wspT", [8, 128, 128])
    bsp = din("bsp", [128, 8])
    w_out = din("w_out", [D, D])
    w_r = din("w_r", [D, NE])
    r_bias = din("r_bias", [1, NE])
    cos_s = din("cos_s", [128, NS, 32])
    sin_s = din("sin_s", [128, NS, 32])
    cos_c = din("cos_c", [128, 32])
    sin_c = din("sin_c", [128, 32])
    cmpmask = din("cmpmask", [128, TOK])
    overlap = din("overlap", [128, 32])
    cst = din("cst", [128, NT, 32])
    emat = din("emat", [32, NS, 128])
    dmask = din("dmask", [128, 128])
    lmask = din("lmask", [128, 128])
    svalid = din("svalid", [128, NS])

    y = nc.dram_tensor("y", [TOK, D], F32, kind="ExternalOutput").ap()
    dbg_outs = {}

    with ExitStack() as es:
        S = Sched(nc, es)
        A = Arena(nc, es)
        psall = es.enter_context(nc.psum_tensor("psall", [128, 4096], F32))
        banks = [Tile(psall[:, i * 512:(i + 1) * 512], Buf("psb%d" % i), 0, 0) for i in range(8)]

        def pair_view(s_):
            return psall[:, (2 * s_) * 512:(2 * s_ + 2) * 512].rearrange("p (b c) -> p b c", c=512)[:, :, 0:256]

        def dbg_dump(name, tile_ap, shape, dt, reads):
            o = nc.dram_tensor("dbg_" + name, list(shape), dt, kind="ExternalOutput").ap()
            b = Buf("dbg_" + name)
            S.dma("sp", o, tile_ap, reads=reads, writes=[b])
            dbg_outs[name] = b

        def finish_all():
            S.finish("sp", list(dbg_outs.values()) + [ybuf])
            for e in ("pe", "act", "dve", "pool"):
                S._wait("sp", e, S.cnt[e])
            for i in range(len(S.dsem)):
                if S.dcnt[i]:
                    S._wait("sp", i, S.dcnt[i])

        ybuf = Buf("y")

        ident = A.alloc("ident", [128], F32)
        S.op("pool", lambda e: e.memset(ident[:], 1.0), writes=[ident.buf])
        S.op("pool", lambda e: e.affine_select(out=ident[:], in_=ident[:], pattern=[[1, 128]],
                                               compare_op=ALU.is_equal, fill=0.0, base=0,
                                               channel_multiplier=-1),
             reads=[ident.buf], writes=[ident.buf])
        identb = A.alloc("identb", [128], BF16)
        S.op("dve", lambda e: e.tensor_copy(out=identb[:], in_=ident[:]), reads=[ident.buf], writes=[identb.buf])
        ones = A.alloc("ones", [128], F32)
        S.op("pool", lambda e: e.memset(ones[:], 1.0), writes=[ones.buf])

        modT = A.alloc("modT", [96], F32)
        gt1b = A.alloc("gt1b", [D], F32)
        gt2b = A.alloc("gt2b", [D], F32)
        gsc1 = A.alloc("gsc1", [DC], F32)
        gsc2 = A.alloc("gsc2", [DC], F32)
        g1t = A.alloc("g1t", [DC], F32)
        g2t = A.alloc("g2t", [DC], F32)
        S.dma("sp", g1t[:], gn1[:, :], writes=[g1t.buf])
        S.dma("sp", g2t[:], gn2[:, :], writes=[g2t.buf])

        ctt = A.alloc("ctt", [DC], F32)
        scb = A.alloc("scb", [DC], BF16)
        S.dma("sp", ctt[:], ct[:, :], writes=[ctt.buf])
        S.op("act", lambda e: e.activation(out=scb[:], in_=ctt[:], func=AF.Silu), reads=[ctt.buf], writes=[scb.buf])
        modrow = A.alloc("modrow", [6 * D], F32, parts=1)
        brow = A.alloc("brow", [6 * D], F32, parts=1)
        S.dma("sp", brow[:], b_ada[0:1, :], writes=[brow.buf])
        wa_ring = Ring([A.alloc("wa%d" % i, [DC, 512], BF16) for i in range(2)])
        for cb in range(24):
            wa = wa_ring.next()
            S.dma("pool", wa[:], w_ada[:, cb * 512:(cb + 1) * 512].rearrange("(c p) f -> p c f", p=128),
                  writes=[wa.buf])
            ps = banks[cb % 2]
            S.mm([(lambda e, k=k, wa=wa, ps=ps: e.matmul(ps[0:1, :], scb[:, k:k + 1], wa[:, k, :],
                                                        start=(k == 0), stop=(k == DC - 1)))
                  for k in range(DC)], reads=[scb.buf, wa.buf], writes=[ps.buf])
            S.op("dve", lambda e, ps=ps, cb=cb: e.tensor_tensor(out=modrow[:, cb * 512:(cb + 1) * 512],
                                                               in0=ps[0:1, :], in1=brow[:, cb * 512:(cb + 1) * 512],
                                                               op=ALU.add),
                 reads=[ps.buf, brow.buf], writes=[modrow.buf])
        ps = banks[2]
        S.mm([(lambda e, j=j: e.matmul(ps[:, j:j + 1], modrow[0:1, j * 128:(j + 1) * 128], ones[0:1, 0:1],
                                       start=True, stop=True)) for j in range(96)],
             reads=[modrow.buf, ones.buf], writes=[ps.buf])
        S.op("dve", lambda e: e.tensor_copy(out=modT[:], in_=ps[:, 0:96]), reads=[ps.buf], writes=[modT.buf])
        S.op("dve", lambda e: e.scalar_tensor_tensor(out=gsc1[:], in0=modT[:, 16:32], scalar=1.0, in1=g1t[:],
                                                     op0=ALU.add, op1=ALU.mult),
             reads=[modT.buf, g1t.buf], writes=[gsc1.buf])
        S.op("dve", lambda e: e.scalar_tensor_tensor(out=gsc2[:], in0=modT[:, 64:80], scalar=1.0, in1=g2t[:],
                                                     op0=ALU.add, op1=ALU.mult),
             reads=[modT.buf, g2t.buf], writes=[gsc2.buf])
        for (dst, base) in ((gt1b, 2 * D), (gt2b, 5 * D)):
            for j in range(4):
                ps = banks[3 + (j % 2)]
                S.mm([lambda e, ps=ps, base=base, j=j: e.matmul(ps[:, :], ones[0:1, :],
                                                               modrow[0:1, base + j * 512: base + (j + 1) * 512],
                                                               start=True, stop=True)],
                     reads=[modrow.buf, ones.buf], writes=[ps.buf])
                S.op("act", lambda e, ps=ps, dst=dst, j=j: e.activation(out=dst[:, j * 512:(j + 1) * 512], in_=ps[:, :],
                                                                       func=AF.Copy),
                     reads=[ps.buf], writes=[dst.buf])
        A.free(modrow, brow, ctt, scb, g1t, g2t, *wa_ring.items)
        if "mod" in dbg:
            dbg_dump("modT", modT[:], [128, 96], F32, [modT.buf])
            dbg_dump("gt1b", gt1b[:], [128, D], F32, [gt1b.buf])
        if stop == "ada":
            finish_all()
            return nc, declared, list(dbg_outs.keys())

        hT = [A.alloc("hT_prev", [DC, TOK], BF16), A.alloc("hT_own", [DC, TOK], BF16)]

        def norm_transpose(xt, xbuf, gsc, shcol0, dst, dst_buf, col0, f32dst=None):
            sq = A.alloc("sq", [D], F32)
            ss = A.alloc("ss", [4], F32)
            S.op("act", lambda e: e.activation(out=sq[:], in_=xt, func=AF.Square), reads=[xbuf], writes=[sq.buf])
            S.op("dve", lambda e: e.reduce_sum(out=ss[:, 0:1], in_=sq[:], axis=AX.X), reads=[sq.buf], writes=[ss.buf])
            S.op("dve", lambda e: e.tensor_scalar(out=ss[:, 1:2], in0=ss[:, 0:1], scalar1=1.0 / D, scalar2=EPS,
                                                  op0=ALU.mult, op1=ALU.add), reads=[ss.buf], writes=[ss.buf])
            S.op("act", lambda e: e.sqrt(out=ss[:, 2:3], in_=ss[:, 1:2]), reads=[ss.buf], writes=[ss.buf])
            S.op("dve", lambda e: e.reciprocal(out=ss[:, 3:4], in_=ss[:, 2:3]), reads=[ss.buf], writes=[ss.buf])
            S.op("dve", lambda e: e.tensor_scalar(out=sq[:], in0=xt, scalar1=ss[:, 3:4], scalar2=None, op0=ALU.mult),
                 reads=[xbuf, ss.buf], writes=[sq.buf])
            for c4 in range(4):
                ps = psring.next()
                S.mm([(lambda e, c=c, ps=ps: e.transpose(ps[:, (c % 4) * 128:(c % 4 + 1) * 128],
                                                         sq[:, c * 128:(c + 1) * 128], ident[:]))
                      for c in range(c4 * 4, c4 * 4 + 4)], reads=[sq.buf, ident.buf], writes=[ps.buf])
                for c in range(c4 * 4, c4 * 4 + 4):
                    tgt = dst[:, c, col0:col0 + 128] if f32dst is None else f32dst[:, c, :]
                    S.op("act", lambda e, c=c, ps=ps, tgt=tgt: e.activation(
                        out=tgt, in_=ps[:, (c % 4) * 128:(c % 4 + 1) * 128], func=AF.Identity,
                        scale=gsc[:, c:c + 1], bias=modT[:, shcol0 + c: shcol0 + c + 1]),
                         reads=[ps.buf, gsc.buf, modT.buf],
                         writes=[dst_buf if f32dst is None else f32dst.buf])
            A.free(sq, ss)

        psring = Ring(banks[0:8])
        xring = Ring([A.alloc("xt%d" % i, [D], F32) for i in range(2)])
        for st in range(NS):
            xt = xring.next()
            S.dma("sp", xt[:], xkv[st * 128:(st + 1) * 128, :], writes=[xt.buf])
            norm_transpose(xt[:], xt.buf, gsc1, 0, hT[st // 8], hT[st // 8].buf, (st % 8) * 128)
        A.free(*xring.items)
        if "hT" in dbg:
            dbg_dump("hT_prev", hT[0][:], [128, DC, TOK], BF16, [hT[0].buf])
            dbg_dump("hT_own", hT[1][:], [128, DC, TOK], BF16, [hT[1].buf])
        if stop == "norm1":
            finish_all()
            return nc, declared, list(dbg_outs.keys())

        def bc_load(name, src, n):
            t = A.alloc(name, [n], F32)
            S.dma("sp", t[:], src[0:1, :].to_broadcast([128, n]), writes=[t.buf])
            return t

        gqb = bc_load("gqb", g_q, HD)
        gkb = bc_load("gkb", g_k, HD)
        ggvb = bc_load("ggvb", g_gv, 1024)
        negM = A.alloc("negM", [4], F32)
        S.op("dve", lambda e: e.reduce_max(out=negM[:, 0:1], in_=gqb[:], axis=AX.X, apply_absolute_value=True),
             reads=[gqb.buf], writes=[negM.buf])
        S.op("dve", lambda e: e.reduce_max(out=negM[:, 1:2], in_=gkb[:], axis=AX.X, apply_absolute_value=True),
             reads=[gkb.buf], writes=[negM.buf])
        S.op("dve", lambda e: e.tensor_tensor(out=negM[:, 2:3], in0=negM[:, 0:1], in1=negM[:, 1:2], op=ALU.mult),
             reads=[negM.buf], writes=[negM.buf])
        S.op("dve", lambda e: e.tensor_scalar(out=negM[:, 3:4], in0=negM[:, 2:3], scalar1=-8.0, scalar2=None, op0=ALU.mult),
             reads=[negM.buf], writes=[negM.buf])
        coss = A.alloc("coss", [NS, 32], F32)
        sins = A.alloc("sins", [NS, 32], F32)
        S.dma("sp", coss[:], cos_s[:, :, :], writes=[coss.buf])
        S.dma("sp", sins[:], sin_s[:, :, :], writes=[sins.buf])
        svt = A.alloc("svt", [NS], F32)
        S.dma("sp", svt[:], svalid[:, :], writes=[svt.buf])
        dmt = A.alloc("dmt", [128], F32)
        S.dma("sp", dmt[:], dmask[:, :], writes=[dmt.buf])
        goutt = A.alloc("goutt", [DC], F32)
        S.dma("sp", goutt[:], gout[:, :], writes=[goutt.buf])
        bspt = A.alloc("bspt", [8], F32)
        S.dma("sp", bspt[:], bsp[:, :], writes=[bspt.buf])

        wring = Ring([A.alloc("wblk%d" % i, [DC, 512], BF16) for i in range(2)])

        def load_wblk(c0, ncols):
            wb = wring.next()
            S.dma("pool", wb[:, :, 0:ncols], w_in[:, c0:c0 + ncols].rearrange("(c p) f -> p c f", p=128),
                  writes=[wb.buf])
            return wb

        def proj(wb, ncols, st):
            ps = psring.next()
            src = hT[st // 8]
            col0 = (st % 8) * 128
            S.mm([(lambda e, k=k: e.matmul(ps[:, 0:ncols], src[:, k, col0:col0 + 128], wb[:, k, 0:ncols],
                                           start=(k == 0), stop=(k == DC - 1))) for k in range(DC)],
                 reads=[src.buf, wb.buf], writes=[ps.buf])
            return ps

        def rstd_from_ss(st_, n, inv_n):
            S.op("dve", lambda e: e.tensor_scalar(out=st_[:, 1, :], in0=st_[:, 0, :], scalar1=inv_n, scalar2=EPS,
                                                  op0=ALU.mult, op1=ALU.add), reads=[st_.buf], writes=[st_.buf])
            S.op("act", lambda e: e.sqrt(out=st_[:, 2, :], in_=st_[:, 1, :]), reads=[st_.buf], writes=[st_.buf])
            S.op("dve", lambda e: e.reciprocal(out=st_[:, 3, :], in_=st_[:, 2, :]), reads=[st_.buf], writes=[st_.buf])

        def norm_rope(src_ap, src_buf, nh, gb, cos_ap, sin_ap, tabs, out_ap, out_buf):
            sq = A.alloc("nr_sq", [nh, HD], F32)
            xn = A.alloc("nr_xn", [nh, HD], F32)
            st_ = A.alloc("nr_st", [4, nh], F32)
            src3 = src_ap.rearrange("p (h d) -> p h d", d=HD)
            S.op("act", lambda e: e.activation(out=sq[:], in_=src3, func=AF.Square), reads=[src_buf], writes=[sq.buf])
            S.op("dve", lambda e: e.reduce_sum(out=st_[:, 0, :], in_=sq[:], axis=AX.X), reads=[sq.buf], writes=[st_.buf])
            rstd_from_ss(st_, nh, 1.0 / HD)
            S.op("dve", lambda e: e.tensor_tensor(out=xn[:], in0=src3,
                                                  in1=st_[:, 3, :].unsqueeze(2).to_broadcast([128, nh, HD]),
                                                  op=ALU.mult), reads=[src_buf, st_.buf], writes=[xn.buf])
            S.op("dve", lambda e: e.tensor_tensor(out=xn[:], in0=xn[:],
                                                  in1=gb[:].unsqueeze(1).to_broadcast([128, nh, HD]),
                                                  op=ALU.mult), reads=[xn.buf, gb.buf], writes=[xn.buf])
            cb = cos_ap.unsqueeze(1).to_broadcast([128, nh, 32])
            sb = sin_ap.unsqueeze(1).to_broadcast([128, nh, 32])
            x1 = xn[:, :, 0:32]
            x2 = xn[:, :, 32:64]
            S.op("dve", lambda e: e.tensor_tensor(out=sq[:, :, 0:32], in0=x1, in1=cb, op=ALU.mult),
                 reads=[xn.buf] + tabs, writes=[sq.buf])
            S.op("dve", lambda e: e.tensor_tensor(out=sq[:, :, 32:64], in0=x2, in1=sb, op=ALU.mult),
                 reads=[xn.buf] + tabs, writes=[sq.buf])
            S.op("dve", lambda e: e.tensor_tensor(out=out_ap[:, :, 0:32], in0=sq[:, :, 0:32], in1=sq[:, :, 32:64],
                                                  op=ALU.subtract), reads=[sq.buf], writes=[out_buf])
            S.op("dve", lambda e: e.tensor_tensor(out=sq[:, :, 0:32], in0=x2, in1=cb, op=ALU.mult),
                 reads=[xn.buf] + tabs, writes=[sq.buf])
            S.op("dve", lambda e: e.tensor_tensor(out=sq[:, :, 32:64], in0=x1, in1=sb, op=ALU.mult),
                 reads=[xn.buf] + tabs, writes=[sq.buf])
            S.op("dve", lambda e: e.tensor_tensor(out=out_ap[:, :, 32:64], in0=sq[:, :, 0:32], in1=sq[:, :, 32:64],
                                                  op=ALU.add), reads=[sq.buf], writes=[out_buf])
            A.free(sq, xn, st_)

        def bank_bf(ps):
            return ps[:, :].bitcast(BF16)

        kcT = A.alloc("kcT", [NKV, SLOTS], BF16)
        vcT = A.alloc("vcT", [NKV, SLOTS], BF16)
        wb = load_wblk(1024, 512)
        for st in range(NS):
            ps = proj(wb, 512, st)
            raw = A.alloc("raw", [512], BF16)
            S.op("act", lambda e: e.activation(out=raw[:], in_=ps[:, :], func=AF.Copy), reads=[ps.buf], writes=[raw.buf])
            pt = psring.next()
            ptb = bank_bf(pt)
            S.mm([(lambda e, j=j: e.transpose(ptb[0:64, j * 128:(j + 1) * 128], raw[:, j * 64:(j + 1) * 64], identb[:]))
                  for j in range(8)], reads=[raw.buf, identb.buf], writes=[pt.buf])
            S.op("dve", lambda e: e.tensor_copy(out=kcT[0:64, :, st * 128:(st + 1) * 128],
                                                in_=ptb[0:64, 0:512].rearrange("p (h t) -> p h t", t=128)),
                 reads=[pt.buf], writes=[kcT.buf])
            S.op("dve", lambda e: e.tensor_copy(out=vcT[0:64, :, st * 128:(st + 1) * 128],
                                                in_=ptb[0:64, 512:1024].rearrange("p (h t) -> p h t", t=128)),
                 reads=[pt.buf], writes=[vcT.buf])
            A.free(raw)
        if "kc" in dbg:
            dbg_dump("kcT", kcT[0:64], [64, NKV, SLOTS], BF16, [kcT.buf])
            dbg_dump("vcT", vcT[0:64], [64, NKV, SLOTS], BF16, [vcT.buf])

        kcmpT2 = A.alloc("kcmpT2", [NKV, 128], BF16)
        vcA = A.alloc("vcA", [NKV, 97], BF16)
        cosc = A.alloc("cosc", [32], F32)
        sinc = A.alloc("sinc", [32], F32)
        ovl = A.alloc("ovl", [32], F32)
        S.dma("sp", cosc[:], cos_c[:, :], writes=[cosc.buf])
        S.dma("sp", sinc[:], sin_c[:, :], writes=[sinc.buf])
        S.dma("sp", ovl[:], overlap[:, :], writes=[ovl.buf])
        S.op("pool", lambda e: e.memset(vcA[:, :, 64:65], 1.0), writes=[vcA.buf])
        S.op("pool", lambda e: e.tensor_copy(out=vcA[:, :, 65:97], in_=ovl[:].unsqueeze(1).to_broadcast([128, NKV, 32])),
             reads=[ovl.buf], writes=[vcA.buf])
        for kind in range(2):
            srcT = kcT if kind == 0 else vcT
            w1t = A.alloc("w1t", [32, 256], BF16)
            w2t = A.alloc("w2t", [2, HD], BF16)
            post = A.alloc("post", [32], F32)
            S.dma("pool", w1t[0:64], (w1k if kind == 0 else w1v).rearrange("(l d) h -> d l h", d=HD), writes=[w1t.buf])
            S.dma("pool", w2t[:], (w2k if kind == 0 else w2v).rearrange("(c p) d -> p c d", p=128), writes=[w2t.buf])
            S.dma("sp", post[0:64], (posk if kind == 0 else posv)[:, :], writes=[post.buf])
            for hk in range(NKV):
                blk = A.alloc("blk", [32, 127], BF16)
                gT = A.alloc("gT", [2, 127], BF16)
                src = srcT[0:64, hk, :].rearrange("p (n l) -> p n l", l=16)
                for half in range(2):
                    S.op("dve", lambda e, half=half: e.tensor_tensor(
                        out=blk[0:64, 16 * half:16 * half + 16, :],
                        in0=src[:, half:half + 127, :].rearrange("p n l -> p l n"),
                        in1=post[0:64, 16 * half:16 * half + 16].unsqueeze(2).to_broadcast([64, 16, 127]),
                        op=ALU.add), reads=[srcT.buf, post.buf], writes=[blk.buf])
                for hc in range(2):
                    pg = psring.next()
                    S.mm([(lambda e, l=l, hc=hc, pg=pg: e.matmul(pg[:, 0:127], w1t[0:64, l, hc * 128:(hc + 1) * 128],
                                                               blk[0:64, l, :], start=(l == 0), stop=(l == 31)))
                          for l in range(32)], reads=[w1t.buf, blk.buf], writes=[pg.buf])
                    S.op("act", lambda e, hc=hc, pg=pg: e.activation(out=gT[:, hc, :], in_=pg[:, 0:127],
                                                                    func=AF.Gelu_apprx_tanh),
                         reads=[pg.buf], writes=[gT.buf])
                po = psring.next()
                S.mm([(lambda e, c=c: e.matmul(po[0:127, 0:HD], gT[:, c, :], w2t[:, c, :], start=(c == 0), stop=(c == 1)))
                      for c in range(2)], reads=[gT.buf, w2t.buf], writes=[po.buf])
                if kind == 0:
                    kn2 = A.alloc("kcn2", [1, 2, HD], BF16)
                    norm_rope(po[:, 0:HD], po.buf, 1, gkb, cosc[:], sinc[:], [cosc.buf, sinc.buf], kn2[:, :, 0, :], kn2.buf)
                    S.op("pool", lambda e: e.tensor_copy(out=kn2[:, :, 1, :], in_=kn2[:, :, 0, :]),
                         reads=[kn2.buf], writes=[kn2.buf])
                    pt = psring.next()
                    ptb = bank_bf(pt)
                    S.mm([lambda e: e.transpose(ptb[:, 0:128], kn2[:, 0, :, :].rearrange("p a d -> p (a d)"), identb[:])],
                         reads=[kn2.buf, identb.buf], writes=[pt.buf])
                    S.op("act", lambda e: e.activation(out=kcmpT2[:, hk, :], in_=ptb[:, 0:128], func=AF.Copy),
                         reads=[pt.buf], writes=[kcmpT2.buf])
                    A.free(kn2)
                else:
                    S.op("act", lambda e: e.activation(out=vcA[0:127, hk, 0:HD], in_=po[0:127, 0:HD], func=AF.Copy),
                         reads=[po.buf], writes=[vcA.buf])
                A.free(blk, gT)
            A.free(w1t, w2t, post)
        A.free(kcT, vcT, cosc, sinc, ovl)
        if "cmp" in dbg:
            dbg_dump("kcmpT2", kcmpT2[:], [128, NKV, 128], BF16, [kcmpT2.buf])
            dbg_dump("vcA", vcA[:], [128, NKV, 97], BF16, [vcA.buf])

        ksT2 = A.alloc("ksT2", [NKV, SLOTS], BF16)
        kwT2 = A.alloc("kwT2", [NKV, SLOTS], BF16)
        vsA = A.alloc("vsA", [NS, NKV, 65], BF16)
        vwA = A.alloc("vwA", [NS, NKV, 65], BF16)
        for (c0, kT2, vA) in ((1536, ksT2, vsA), (2048, kwT2, vwA)):
            wb = load_wblk(c0, 512)
            ps_next = proj(wb, 512, 0)
            for st in range(NS):
                ps = ps_next
                kn2 = A.alloc("kn2", [NKV, 2, HD], BF16)
                norm_rope(ps[:, 0:256], ps.buf, NKV, gkb, coss[:, st, :], sins[:, st, :], [coss.buf, sins.buf],
                          kn2[:, :, 0, :], kn2.buf)
                S.op("pool", lambda e: e.tensor_copy(out=kn2[:, :, 1, :], in_=kn2[:, :, 0, :]),
                     reads=[kn2.buf], writes=[kn2.buf])
                S.op("dve", lambda e: e.tensor_scalar(out=vA[:, st, :, 0:64],
                                                      in0=ps[:, 256:512].rearrange("p (h d) -> p h d", d=HD),
                                                      scalar1=svt[:, st:st + 1], scalar2=None, op0=ALU.mult),
                     reads=[ps.buf, svt.buf], writes=[vA.buf])
                S.op("dve", lambda e: e.tensor_copy(out=vA[:, st, :, 64:65],
                                                    in_=svt[:, st:st + 1].unsqueeze(1).to_broadcast([128, NKV, 1])),
                     reads=[svt.buf], writes=[vA.buf])
                if st + 1 < NS:
                    ps_next = proj(wb, 512, st + 1)
                pt = psring.next()
                ptb = bank_bf(pt)
                S.mm([(lambda e, h=h: e.transpose(ptb[:, h * 128:(h + 1) * 128],
                                                  kn2[:, h, :, :].rearrange("p a d -> p (a d)"), identb[:]))
                      for h in range(NKV)], reads=[kn2.buf, identb.buf], writes=[pt.buf])
                S.op("act", lambda e: e.activation(out=kT2[:, :, st * 128:(st + 1) * 128],
                                                   in_=ptb[:, 0:512].rearrange("p (h t) -> p h t", t=128), func=AF.Copy),
                     reads=[pt.buf], writes=[kT2.buf])
                A.free(kn2)
        A.free(hT[0])
        if "ks" in dbg:
            dbg_dump("ksT2", ksT2[:], [128, NKV, SLOTS], BF16, [ksT2.buf])
            dbg_dump("kwT2", kwT2[:], [128, NKV, SLOTS], BF16, [kwT2.buf])
            dbg_dump("vsA", vsA[:], [128, NS, NKV, 65], BF16, [vsA.buf])
            dbg_dump("vwA", vwA[:], [128, NS, NKV, 65], BF16, [vwA.buf])
        if stop == "kv":
            finish_all()
            return nc, declared, list(dbg_outs.keys())

        qT = A.alloc("qT", [8, TOK], BF16)
        for qb in range(2):
            wb = load_wblk(qb * 512, 512)
            ps_next = proj(wb, 512, 8)
            for i in range(NT):
                ps = ps_next
                qn = A.alloc("qn", [8, HD], BF16)
                norm_rope(ps[:, :], ps.buf, 8, gqb, coss[:, 8 + i, :], sins[:, 8 + i, :], [coss.buf, sins.buf],
                          qn[:], qn.buf)
                if i + 1 < NT:
                    ps_next = proj(wb, 512, 9 + i)
                pt = psring.next()
                ptb = bank_bf(pt)
                S.mm([(lambda e, j=j: e.transpose(ptb[:, j * 128:(j + 1) * 128],
                                                  qn[:, 2 * j:2 * j + 2, :].rearrange("p a d -> p (a d)"), identb[:]))
                      for j in range(4)], reads=[qn.buf, identb.buf], writes=[pt.buf])
                S.op("act", lambda e: e.activation(out=qT[:, 4 * qb:4 * qb + 4, i * 128:(i + 1) * 128],
                                                   in_=ptb[:, 0:512].rearrange("p (h t) -> p h t", t=128), func=AF.Copy),
                     reads=[pt.buf], writes=[qT.buf])
                A.free(qn)
        gsig = A.alloc("gsig", [NT, 3, NH], F32)
        wb = load_wblk(2560, 48)
        for i in range(NT):
            ps = proj(wb, 48, 8 + i)
            S.op("act", lambda e: e.activation(out=gsig[:, i, :, :], in_=ps[:, 0:48].rearrange("p (h r) -> p r h", r=3),
                                               func=AF.Sigmoid),
                 reads=[ps.buf], writes=[gsig.buf])
        if "q" in dbg:
            dbg_dump("qT", qT[:], [128, 8, TOK], BF16, [qT.buf])
            dbg_dump("gsig", gsig[:], [128, NT, 3, NH], F32, [gsig.buf])
        if stop == "q":
            finish_all()
            return nc, declared, list(dbg_outs.keys())

        A.free(wring.items[1], coss, sins, gqb, gkb)
        wring = Ring([wring.items[0]])
        catTg = A.alloc("catTg", [8, TOK], BF16)
        ssg = A.alloc("ssg", [NT, 2], F32)
        wspf = A.alloc("wspf", [8, 128], F32)
        wsp = A.alloc("wsp", [8, 128], BF16)
        S.dma("sp", wspf[:], wspT.rearrange("g s t -> s g t"), writes=[wspf.buf])
        S.op("dve", lambda e: e.tensor_tensor(out=wsp[:], in0=wspf[:], in1=dmt[:].unsqueeze(1).to_broadcast([128, 8, 128]),
                                              op=ALU.mult), reads=[wspf.buf, dmt.buf], writes=[wsp.buf])
        gu = A.alloc("gu", [NT, 1024], BF16)
        for ub in range(2):
            wb = load_wblk(2608 + ub * 512, 512)
            for i in range(NT):
                ps = proj(wb, 512, 8 + i)
                S.op("act", lambda e: e.activation(out=gu[:, i, ub * 512:(ub + 1) * 512], in_=ps[:, :],
                                                   func=AF.Gelu_apprx_tanh), reads=[ps.buf], writes=[gu.buf])
        for vb in range(2):
            wb = load_wblk(3632 + vb * 512, 512)
            ps_next = proj(wb, 512, 8)
            for i in range(NT):
                ps = ps_next
                gv = A.alloc("gv", [4, 128], F32)
                sq = A.alloc("gsq", [4, 128], F32)
                st_ = A.alloc("gst", [4, 4], F32)
                vn = A.alloc("vn", [4, 128], BF16)
                og = A.alloc("og", [4, 128], F32)
                S.op("act", lambda e: e.activation(out=gv[:], in_=ps[:, :].rearrange("p (g d) -> p g d", d=128),
                                                   func=AF.Gelu_apprx_tanh), reads=[ps.buf], writes=[gv.buf])
                S.op("act", lambda e: e.activation(out=sq[:], in_=gv[:], func=AF.Square), reads=[gv.buf], writes=[sq.buf])
                S.op("dve", lambda e: e.reduce_sum(out=st_[:, 0, :], in_=sq[:], axis=AX.X), reads=[sq.buf], writes=[st_.buf])
                rstd_from_ss(st_, 4, 1.0 / 128)
                S.op("dve", lambda e: e.tensor_tensor(out=gv[:], in0=gv[:],
                                                      in1=st_[:, 3, :].unsqueeze(2).to_broadcast([128, 4, 128]),
                                                      op=ALU.mult), reads=[gv.buf, st_.buf], writes=[gv.buf])
                S.op("dve", lambda e: e.tensor_tensor(out=vn[:], in0=gv[:],
                                                      in1=ggvb[:, vb * 512:(vb + 1) * 512].rearrange("p (g d) -> p g d", d=128),
                                                      op=ALU.mult), reads=[gv.buf, ggvb.buf], writes=[vn.buf])
                if i + 1 < NT:
                    ps_next = proj(wb, 512, 9 + i)
                p2 = psring.next()
                S.mm([(lambda e, g=g: e.matmul(p2[:, g * 128:(g + 1) * 128], wsp[:, 4 * vb + g, :], vn[:, g, :],
                                               start=True, stop=True)) for g in range(4)],
                     reads=[wsp.buf, vn.buf], writes=[p2.buf])
                for g in range(4):
                    G = 4 * vb + g
                    S.op("dve", lambda e, g=g, G=G: e.scalar_tensor_tensor(
                        out=og[:, g, :], in0=p2[:, g * 128:(g + 1) * 128], scalar=bspt[:, G:G + 1],
                        in1=gu[:, i, G * 128:(G + 1) * 128], op0=ALU.add, op1=ALU.mult),
                         reads=[p2.buf, bspt.buf, gu.buf], writes=[og.buf])
                S.op("act", lambda e: e.activation(out=sq[:], in_=og[:], func=AF.Square), reads=[og.buf], writes=[sq.buf])
                S.op("dve", lambda e: e.reduce_sum(out=ssg[:, i, vb:vb + 1], in_=sq[:].rearrange("p g d -> p (g d)"), axis=AX.X),
                     reads=[sq.buf], writes=[ssg.buf])
                p3 = psring.next()
                S.mm([(lambda e, g=g: e.transpose(p3[:, g * 128:(g + 1) * 128], og[:, g, :], ident[:])) for g in range(4)],
                     reads=[og.buf, ident.buf], writes=[p3.buf])
                for g in range(4):
                    c = 8 + 4 * vb + g
                    S.op("act", lambda e, g=g, c=c: e.activation(out=catTg[:, c - 8, i * 128:(i + 1) * 128],
                                                                 in_=p3[:, g * 128:(g + 1) * 128], func=AF.Copy,
                                                                 scale=goutt[:, c:c + 1]),
                         reads=[p3.buf, goutt.buf], writes=[catTg.buf])
                A.free(gv, sq, st_, vn, og)
        A.free(gu, wspf, wsp, hT[1], ggvb, *wring.items)
        if "gmlp" in dbg:
            dbg_dump("catTg", catTg[:], [128, 8, TOK], BF16, [catTg.buf])
            dbg_dump("ssg", ssg[:], [128, NT, 2], F32, [ssg.buf])
        if stop == "gmlp":
            finish_all()
            return nc, declared, list(dbg_outs.keys())

        cmk = A.alloc("cmk", [TOK], BF16)
        S.dma("pool", cmk[:], cmpmask[:, :], writes=[cmk.buf])
        cstt = A.alloc("cstt", [NT, 32], F32)
        S.dma("sp", cstt[:], cst[:, :, :], writes=[cstt.buf])
        emt = A.alloc("emt", [NS, 128], BF16)
        S.dma("pool", emt[0:32], emat[:, :, :], writes=[emt.buf])
        S.dma("pool", emt[64:96], emat[:, :, :], writes=[emt.buf])
        dmb = A.alloc("dmb", [128], BF16)
        lmb = A.alloc("lmb", [128], BF16)
        S.op("dve", lambda e: e.tensor_copy(out=dmb[:], in_=dmt[:]), reads=[dmt.buf], writes=[dmb.buf])
        S.dma("pool", lmb[:], lmask[:, :], writes=[lmb.buf])
        selT = A.alloc("selT", [NKV, TOK], BF16)
        onsa = A.alloc("onsa", [NT, 1024], F32)
        S.op("pool", lambda e: e.memset(onsa[:], 0.0), writes=[onsa.buf])
        Sring = Ring([(0, banks[0], banks[1]), (1, banks[2], banks[3]), (2, banks[4], banks[5])])
        accring = Ring(banks[6:8])
        PTring = Ring([A.alloc("pt%d" % i, [512], BF16) for i in range(6)])

        def qk_fns(pr, kT2, hk, ksl, kw_, i, bias_kt=None):
            fns = []
            for half, pb in ((0, 0), (1, 64)):
                bank = pr[1 + half]
                if bias_kt is not None:
                    fns.append(lambda e, bank=bank, pb=pb: e.matmul(
                        bank[:, 0:256], emt[pb:pb + 32, bias_kt, :],
                        selT[pb:pb + 32, hk, i * 128:(i + 1) * 128].unsqueeze(1).to_broadcast([32, 2, 128]),
                        start=True, stop=False))
                fns.append(lambda e, bank=bank, pb=pb: e.matmul(
                    bank[0:kw_, 0:256], kT2[pb:pb + 64, hk, ksl],
                    qT[pb:pb + 64, 2 * hk:2 * hk + 2, i * 128:(i + 1) * 128],
                    start=(bias_kt is None), stop=True))
            return fns

        def exp_pair(pr, pt, kp):
            S.op("act", lambda e: e.activation(out=pt[0:kp, :].rearrange("p (b c) -> p b c", c=256),
                                               in_=pair_view(pr[0])[0:kp], func=AF.Exp, scale=0.125,
                                               bias=(negM[0:kp, 3:4] if os.environ.get("STAB", "1") == "1" else 0.0)),
                 reads=[pr[1].buf, pr[2].buf, negM.buf], writes=[pt.buf])

        def evac(acc, hk, i, br, want_imp=False):
            r4 = A.alloc("r4", [4], F32)
            cf = A.alloc("cf", [4], F32)
            accv = acc[:, :].rearrange("p (j w) -> p j w", w=128)
            S.op("dve", lambda e: e.tensor_scalar(out=r4[:], in0=accv[:, :, 64], scalar1=1e-30, scalar2=None, op0=ALU.max),
                 reads=[acc.buf], writes=[r4.buf])
            S.op("dve", lambda e: e.reciprocal(out=r4[:], in_=r4[:]), reads=[r4.buf], writes=[r4.buf])
            S.op("dve", lambda e: e.tensor_tensor(out=cf[:].rearrange("p (b a) -> p b a", a=2),
                                                  in0=r4[:].rearrange("p (a b) -> p b a", b=2),
                                                  in1=gsig[:, i, br, 4 * hk:4 * hk + 4].rearrange("p (b a) -> p b a", a=2),
                                                  op=ALU.mult), reads=[r4.buf, gsig.buf], writes=[cf.buf])
            imp = None
            if want_imp:
                imp = A.alloc("imp", [32], F32)
                for j in range(4):
                    if j == 0:
                        S.op("dve", lambda e: e.tensor_scalar(out=imp[:], in0=acc[:, 65:97], scalar1=r4[:, 0:1], scalar2=None,
                                                              op0=ALU.mult), reads=[acc.buf, r4.buf], writes=[imp.buf])
                    else:
                        S.op("dve", lambda e, j=j: e.scalar_tensor_tensor(out=imp[:], in0=acc[:, j * 128 + 65:j * 128 + 97],
                                                                         scalar=r4[:, j:j + 1], in1=imp[:],
                                                                         op0=ALU.mult, op1=ALU.add),
                             reads=[acc.buf, r4.buf, imp.buf], writes=[imp.buf])
            for j in range(4):
                g = GORDER[j]
                h = 4 * hk + g
                S.op("dve", lambda e, j=j, g=g, h=h: e.scalar_tensor_tensor(
                    out=onsa[:, i, h * 64:(h + 1) * 64], in0=acc[:, j * 128:j * 128 + 64], scalar=cf[:, g:g + 1],
                    in1=onsa[:, i, h * 64:(h + 1) * 64], op0=ALU.mult, op1=ALU.add),
                     reads=[acc.buf, cf.buf, onsa.buf], writes=[onsa.buf])
            A.free(r4, cf)
            return imp

        def pv(acc, pt, kp, vA_ap, vbuf, width, first, last):
            S.mm([(lambda e, j=j: e.matmul(acc[:, j * 128:j * 128 + width], pt[0:kp, j * 128:(j + 1) * 128], vA_ap,
                                           start=(first and j == 0), stop=last, skip_group_check=True)) for j in range(4)],
                 reads=[pt.buf, vbuf], writes=[acc.buf])

        def mask_mul(pt, kp, mk_ap, mbuf):
            S.op("pool", lambda e: e.tensor_tensor(out=pt[0:kp, :].rearrange("p (j q) -> p j q", q=128),
                                                   in0=pt[0:kp, :].rearrange("p (j q) -> p j q", q=128),
                                                   in1=mk_ap.unsqueeze(1).to_broadcast([kp, 4, 128]), op=ALU.mult),
                 reads=[pt.buf, mbuf], writes=[pt.buf])

        units = []
        for hk in range(NKV):
            for i in range(NT):
                units.append(dict(kind="cmp", hk=hk, i=i, kt=0, first=True, last=True, grp={}))
                g = {}
                for kt in range(4 + i, 9 + i):
                    units.append(dict(kind="win", hk=hk, i=i, kt=kt, first=(kt == 4 + i), last=(kt == 8 + i), grp=g))
                g = {}
                for kt in range(0, 9 + i):
                    units.append(dict(kind="sel", hk=hk, i=i, kt=kt, first=(kt == 0), last=(kt == 8 + i), grp=g))

        def u_qk(u):
            hk, i, kt = u["hk"], u["i"], u["kt"]
            pr = Sring.next()
            u["pr"] = pr
            if u["kind"] == "cmp":
                S.mm(qk_fns(pr, kcmpT2, hk, slice(0, 127), 127, i), reads=[kcmpT2.buf, qT.buf], writes=[pr[1].buf, pr[2].buf])
            elif u["kind"] == "win":
                S.mm(qk_fns(pr, kwT2, hk, slice(kt * 128, (kt + 1) * 128), 128, i),
                     reads=[kwT2.buf, qT.buf], writes=[pr[1].buf, pr[2].buf])
            else:
                S.mm(qk_fns(pr, ksT2, hk, slice(kt * 128, (kt + 1) * 128), 128, i, bias_kt=kt),
                     reads=[emt.buf, selT.buf, ksT2.buf, qT.buf], writes=[pr[1].buf, pr[2].buf])

        def u_post(u):
            hk, i, kt, kind = u["hk"], u["i"], u["kt"], u["kind"]
            qsl = slice(i * 128, (i + 1) * 128)
            pr = u["pr"]
            pt = PTring.next()
            kp = 127 if kind == "cmp" else 128
            exp_pair(pr, pt, kp)
            if kind == "cmp":
                mask_mul(pt, 127, cmk[0:127, qsl], cmk.buf)
            elif kind == "win":
                if kt == 4 + i:
                    mask_mul(pt, 128, lmb[:], lmb.buf)
                if kt == 8 + i:
                    mask_mul(pt, 128, dmb[:], dmb.buf)
            else:
                if kt == 8 + i:
                    mask_mul(pt, 128, dmb[:], dmb.buf)
            if u["first"]:
                u["grp"]["acc"] = accring.next()
            acc = u["grp"]["acc"]
            if kind == "cmp":
                pv(acc, pt, 127, vcA[0:127, hk, :], vcA.buf, 97, True, True)
            elif kind == "win":
                pv(acc, pt, 128, vwA[:, kt, hk, :], vwA.buf, 65, u["first"], u["last"])
            else:
                pv(acc, pt, 128, vsA[:, kt, hk, :], vsA.buf, 65, u["first"], u["last"])
            if not u["last"]:
                return
            if kind == "win":
                evac(acc, hk, i, 2)
            elif kind == "sel":
                evac(acc, hk, i, 1)
            else:
                imp = evac(acc, hk, i, 0, want_imp=True)
                sc = A.alloc("sc", [32], F32)
                wk_ = A.alloc("wk", [32], F32)
                m8 = A.alloc("m8", [16], F32)
                sm1 = A.alloc("sm1", [96], BF16)
                S.op("pool", lambda e: e.memset(sm1[:, 32:64], 0.0), writes=[sm1.buf])
                S.op("dve", lambda e: e.tensor_tensor(out=sc[:], in0=imp[:], in1=cstt[:, i, :], op=ALU.add),
                     reads=[imp.buf, cstt.buf], writes=[sc.buf])
                S.op("dve", lambda e: e.max(out=m8[:, 0:8], in_=sc[:]), reads=[sc.buf], writes=[m8.buf])
                S.op("dve", lambda e: e.match_replace(out=wk_[:], in_to_replace=m8[:, 0:8], in_values=sc[:], imm_value=-1e30),
                     reads=[sc.buf, m8.buf], writes=[wk_.buf])
                S.op("dve", lambda e: e.max(out=m8[:, 8:16], in_=wk_[:]), reads=[wk_.buf], writes=[m8.buf])
                S.op("dve", lambda e: e.tensor_scalar(out=m8[:, 0:1], in0=m8[:, 15:16], scalar1=0.0, scalar2=None, op0=ALU.max),
                     reads=[m8.buf], writes=[m8.buf])
                S.op("dve", lambda e: e.tensor_scalar(out=sm1[:, 0:32], in0=sc[:], scalar1=m8[:, 0:1], scalar2=-1.0,
                                                      op0=ALU.is_ge, op1=ALU.add), reads=[sc.buf, m8.buf], writes=[sm1.buf])
                S.op("dve", lambda e: e.tensor_copy(out=sm1[:, 64:96], in_=sm1[:, 0:32]), reads=[sm1.buf], writes=[sm1.buf])
                pm = accring.items[accring.i]
                pmb = bank_bf(pm)
                S.mm([lambda e: e.transpose(pmb[0:96, 0:128], sm1[:], identb[:])], reads=[sm1.buf, identb.buf], writes=[pm.buf])
                S.op("act", lambda e: e.activation(out=selT[0:96, hk, qsl], in_=pmb[0:96, 0:128], func=AF.Copy),
                     reads=[pm.buf], writes=[selT.buf])
                A.free(imp, sc, wk_, m8, sm1)

        DEPTH = 2
        for n in range(min(DEPTH, len(units))):
            u_qk(units[n])
        for n in range(len(units)):
            if n + DEPTH < len(units):
                u_qk(units[n + DEPTH])
            u_post(units[n])
        A.free(cmk, cstt, emt, dmb, lmb, selT, qT, ksT2, kwT2, vsA, vwA, kcmpT2, vcA, gsig, negM, *PTring.items)

        catTn = A.alloc("catTn", [8, TOK], BF16)
        ssn = A.alloc("ssn", [NT], F32)
        for i in range(NT):
            sq = A.alloc("osq", [1024], F32)
            S.op("act", lambda e: e.activation(out=sq[:], in_=onsa[:, i, :], func=AF.Square), reads=[onsa.buf], writes=[sq.buf])
            S.op("dve", lambda e: e.reduce_sum(out=ssn[:, i:i + 1], in_=sq[:], axis=AX.X), reads=[sq.buf], writes=[ssn.buf])
            A.free(sq)
            for c4 in range(2):
                p3 = psring.next()
                S.mm([(lambda e, c=c: e.transpose(p3[:, (c % 4) * 128:(c % 4 + 1) * 128], onsa[:, i, c * 128:(c + 1) * 128], ident[:]))
                      for c in range(4 * c4, 4 * c4 + 4)], reads=[onsa.buf, ident.buf], writes=[p3.buf])
                for c in range(4 * c4, 4 * c4 + 4):
                    S.op("act", lambda e, c=c, p3=p3: e.activation(out=catTn[:, c, i * 128:(i + 1) * 128],
                                                                   in_=p3[:, (c % 4) * 128:(c % 4 + 1) * 128], func=AF.Copy,
                                                                   scale=goutt[:, c:c + 1]),
                         reads=[p3.buf, goutt.buf], writes=[catTn.buf])
        if "nsa" in dbg:
            dbg_dump("onsa", onsa[:], [128, NT, 1024], F32, [onsa.buf])
            dbg_dump("catTn", catTn[:], [128, 8, TOK], BF16, [catTn.buf])
            dbg_dump("ssn", ssn[:], [128, NT], F32, [ssn.buf])
        A.free(onsa)
        if stop == "nsa":
            finish_all()
            return nc, declared, list(dbg_outs.keys())

        A.free(dmt, bspt, svt)
        wo = A.alloc("wo", [DC, D], BF16)
        for nb in range(4):
            S.dma("pool", wo[:, :, nb * 512:(nb + 1) * 512],
                  w_out[:, nb * 512:(nb + 1) * 512].rearrange("(c p) f -> p c f", p=128), writes=[wo.buf])
        stn = A.alloc("stn", [4, NT], F32)
        stg = A.alloc("stg", [4, NT], F32)
        S.op("dve", lambda e: e.tensor_copy(out=stn[:, 0, :], in_=ssn[:]), reads=[ssn.buf], writes=[stn.buf])
        S.op("dve", lambda e: e.tensor_tensor(out=stg[:, 0, :], in0=ssg[:, :, 0], in1=ssg[:, :, 1], op=ALU.add),
             reads=[ssg.buf], writes=[stg.buf])
        rstd_from_ss(stn, NT, 1.0 / 1024)
        rstd_from_ss(stg, NT, 1.0 / 1024)
        h2T = A.alloc("h2T", [DC, TOK], BF16)
        wT = A.alloc("wT", [TOK], F32)
        wrt = A.alloc("wrt", [DC, NE], F32)
        S.dma("sp", wrt[:], w_r.rearrange("(c p) e -> p c e", p=128), writes=[wrt.buf])
        rbb = bc_load("rbb", r_bias, NE)
        ybufs = [Buf("y%d" % i) for i in range(NT)]
        for i in range(NT):
            tsl = slice(i * 128, (i + 1) * 128)
            xt = A.alloc("xt5", [D], F32)
            x1 = A.alloc("x1", [D], F32)
            S.dma("sp", xt[:], xkv[(8 + i) * 128:(9 + i) * 128, :], writes=[xt.buf])
            for nb in range(4):
                nsl = slice(nb * 512, (nb + 1) * 512)
                pa = psring.next()
                S.mm([(lambda e, c=c: e.matmul(pa[:, :], catTn[:, c, tsl], wo[:, c, nsl], start=(c == 0), stop=(c == 7)))
                      for c in range(8)], reads=[catTn.buf, wo.buf], writes=[pa.buf])
                pb_ = psring.next()
                S.mm([(lambda e, c=c: e.matmul(pb_[:, :], catTg[:, c, tsl], wo[:, 8 + c, nsl], start=(c == 0), stop=(c == 7)))
                      for c in range(8)], reads=[catTg.buf, wo.buf], writes=[pb_.buf])
                tmp = A.alloc("mixtmp", [512], F32)
                S.op("dve", lambda e: e.tensor_scalar(out=tmp[:], in0=pa[:, :], scalar1=stn[:, 3, i:i + 1], scalar2=None,
                                                      op0=ALU.mult), reads=[pa.buf, stn.buf], writes=[tmp.buf])
                S.op("dve", lambda e: e.scalar_tensor_tensor(out=tmp[:], in0=pb_[:, :], scalar=stg[:, 3, i:i + 1], in1=tmp[:],
                                                             op0=ALU.mult, op1=ALU.add),
                     reads=[pb_.buf, stg.buf, tmp.buf], writes=[tmp.buf])
                S.op("dve", lambda e: e.tensor_tensor(out=tmp[:], in0=tmp[:], in1=gt1b[:, nsl], op=ALU.mult),
                     reads=[tmp.buf, gt1b.buf], writes=[tmp.buf])
                S.op("dve", lambda e: e.tensor_tensor(out=x1[:, nsl], in0=tmp[:], in1=xt[:, nsl], op=ALU.add),
                     reads=[tmp.buf, xt.buf], writes=[x1.buf])
                A.free(tmp)
            S.dma("sp", y[tsl, :], x1[:], reads=[x1.buf], writes=[ybufs[i]])
            h2f = A.alloc("h2f", [DC, 128], F32)
            norm_transpose(x1[:], x1.buf, gsc2, 48, None, None, 0, f32dst=h2f)
            S.op("pool", lambda e: e.tensor_copy(out=h2T[:, :, tsl], in_=h2f[:]), reads=[h2f.buf], writes=[h2T.buf])
            pl = psring.next()
            S.mm([(lambda e, c=c: e.matmul(pl[:, 0:NE], h2f[:, c, :], wrt[:, c, :], start=(c == 0), stop=(c == DC - 1)))
                  for c in range(DC)], reads=[h2f.buf, wrt.buf], writes=[pl.buf])
            r_s = A.alloc("r_s", [NE], F32)
            r_sb = A.alloc("r_sb", [NE], F32)
            r_g8 = A.alloc("r_g8", [8, 8], F32)
            r_m = A.alloc("r_m", [4, 8], F32)
            r_w = A.alloc("r_w", [NE], F32)
            S.op("act", lambda e: e.activation(out=r_s[:], in_=pl[:, 0:NE], func=AF.Sigmoid), reads=[pl.buf], writes=[r_s.buf])
            S.op("dve", lambda e: e.tensor_tensor(out=r_sb[:], in0=r_s[:], in1=rbb[:], op=ALU.add),
                 reads=[r_s.buf, rbb.buf], writes=[r_sb.buf])
            for g in range(8):
                S.op("dve", lambda e, g=g: e.max(out=r_g8[:, g, :], in_=r_sb[:, g * 8:(g + 1) * 8]),
                     reads=[r_sb.buf], writes=[r_g8.buf])
            S.op("dve", lambda e: e.tensor_tensor(out=r_m[:, 0, :], in0=r_g8[:, :, 0], in1=r_g8[:, :, 1], op=ALU.add),
                 reads=[r_g8.buf], writes=[r_m.buf])
            S.op("dve", lambda e: e.max(out=r_m[:, 1, :], in_=r_m[:, 0, :]), reads=[r_m.buf], writes=[r_m.buf])
            S.op("dve", lambda e: e.tensor_scalar(out=r_m[:, 2, :], in0=r_m[:, 0, :], scalar1=r_m[:, 1, 3:4], scalar2=None,
                                                  op0=ALU.is_ge), reads=[r_m.buf], writes=[r_m.buf])
            S.op("dve", lambda e: e.tensor_scalar(out=r_m[:, 3, :], in0=r_m[:, 2, :], scalar1=4.0, scalar2=-4.0,
                                                  op0=ALU.mult, op1=ALU.add), reads=[r_m.buf], writes=[r_m.buf])
            sb3 = r_sb[:].rearrange("p (g k) -> p g k", k=8)
            S.op("dve", lambda e: e.tensor_tensor(out=sb3, in0=sb3, in1=r_m[:, 2, :].unsqueeze(2).to_broadcast([128, 8, 8]),
                                                  op=ALU.mult), reads=[r_sb.buf, r_m.buf], writes=[r_sb.buf])
            S.op("dve", lambda e: e.tensor_tensor(out=sb3, in0=sb3, in1=r_m[:, 3, :].unsqueeze(2).to_broadcast([128, 8, 8]),
                                                  op=ALU.add), reads=[r_sb.buf, r_m.buf], writes=[r_sb.buf])
            S.op("dve", lambda e: e.max(out=r_m[:, 1, :], in_=r_sb[:]), reads=[r_sb.buf, r_m.buf], writes=[r_m.buf])
            S.op("dve", lambda e: e.tensor_scalar(out=r_w[:], in0=r_sb[:], scalar1=r_m[:, 1, 7:8], scalar2=None, op0=ALU.is_ge),
                 reads=[r_sb.buf, r_m.buf], writes=[r_w.buf])
            S.op("dve", lambda e: e.tensor_tensor(out=r_w[:], in0=r_w[:], in1=r_s[:], op=ALU.mult),
                 reads=[r_w.buf, r_s.buf], writes=[r_w.buf])
            S.op("dve", lambda e: e.reduce_sum(out=r_m[:, 0, 0:1], in_=r_w[:], axis=AX.X), reads=[r_w.buf, r_m.buf], writes=[r_m.buf])
            S.op("dve", lambda e: e.reciprocal(out=r_m[:, 0, 1:2], in_=r_m[:, 0, 0:1]), reads=[r_m.buf], writes=[r_m.buf])
            S.op("dve", lambda e: e.tensor_scalar(out=r_w[:], in0=r_w[:], scalar1=r_m[:, 0, 1:2], scalar2=2.5,
                                                  op0=ALU.mult, op1=ALU.mult), reads=[r_w.buf, r_m.buf], writes=[r_w.buf])
            pt_ = psring.next()
            S.mm([lambda e: e.transpose(pt_[0:NE, 0:128], r_w[:], ident[:])], reads=[r_w.buf, ident.buf], writes=[pt_.buf])
            S.op("act", lambda e: e.activation(out=wT[0:NE, tsl], in_=pt_[0:NE, 0:128], func=AF.Copy),
                 reads=[pt_.buf], writes=[wT.buf])
            A.free(xt, x1, h2f, r_s, r_sb, r_g8, r_m, r_w)
        A.free(wo, catTn, catTg, stn, stg, ssn, ssg, wrt, rbb, gt1b, goutt)
        if "x1" in dbg:
            dbg_dump("wT", wT[0:NE], [NE, TOK], F32, [wT.buf])
            dbg_dump("h2T", h2T[:], [128, DC, TOK], BF16, [h2T.buf])
        if stop == "x1":
            finish_all_y = list(ybufs)
            S.finish("sp", finish_all_y)
            finish_all()
            return nc, declared, list(dbg_outs.keys())

        w_gate = din("w_gate", [NE, D, 512])
        w_up = din("w_up", [NE, D, 512])
        w_down = din("w_down", [NE, 512, D])
        ws_gate = din("ws_gate", [D, 512])
        ws_up = din("ws_up", [D, 512])
        ws_down = din("ws_down", [512, D])
        acc = A.alloc("acc", [NT, D], F32)
        accb = [[Buf("acc%d_%d" % (i, nb)) for nb in range(4)] for i in range(NT)]
        for i in range(NT):
            S.op("pool", lambda e, i=i: e.memset(acc[:, i, :], 0.0), writes=[acc.buf] + accb[i])
        wgu_ring = Ring([A.alloc("wgu%d" % i, [2, DC, 256], BF16) for i in range(2)])
        wd_ring = Ring([A.alloc("wd%d" % i, [4, D], BF16) for i in range(2)])
        hid_ring = Ring([A.alloc("hidT%d" % i, [4, TOK], BF16) for i in range(2)])
        wbs = A.alloc("wbs", [TOK], F32)
        sg_ring = Ring([A.alloc("sg%d" % i, [512], F32) for i in range(2)])
        tt_ring = Ring([A.alloc("tt%d" % i, [512], F32) for i in range(2)])
        gu_ring = Ring(banks[0:4])
        out_ring = Ring(banks[4:7])
        elist = list(range(NE)) + [NE]

        def gu_phase(e_):
            shared = (e_ == NE)
            halves = []
            for fh in range(2):
                wgu = wgu_ring.next()
                gsrc = ws_gate if shared else w_gate[e_]
                usrc = ws_up if shared else w_up[e_]
                S.dma("pool", wgu[:, 0, :, :], gsrc[:, fh * 256:(fh + 1) * 256].rearrange("(c p) f -> p c f", p=128),
                      writes=[wgu.buf])
                S.dma("pool", wgu[:, 1, :, :], usrc[:, fh * 256:(fh + 1) * 256].rearrange("(c p) f -> p c f", p=128),
                      writes=[wgu.buf])
                halves.append(wgu)
            wd_t = wd_ring.next()
            S.dma("pool", wd_t[:], (ws_down if shared else w_down[e_]).rearrange("(c p) d -> p c d", p=128), writes=[wd_t.buf])
            if not shared:
                pw = banks[7]
                for half in range(2):
                    S.mm([lambda e, half=half: e.matmul(pw[:, :], ident[0:NE, e_:e_ + 1].to_broadcast([NE, 128]),
                                                       wT[0:NE, half * 512:(half + 1) * 512], start=True, stop=True)],
                         reads=[ident.buf, wT.buf], writes=[pw.buf])
                    S.op("act", lambda e, half=half: e.activation(out=wbs[:, half * 512:(half + 1) * 512], in_=pw[:, :],
                                                                  func=AF.Copy), reads=[pw.buf], writes=[wbs.buf])
            hidT = hid_ring.next()
            for fh in range(2):
                wgu = halves[fh]
                for fcl in range(2):
                    fc = 2 * fh + fcl
                    for tc in range(2):
                        csl = slice(tc * 512, (tc + 1) * 512)
                        pg = gu_ring.next()
                        S.mm([(lambda e, k=k, pg=pg: e.matmul(pg[:, :], wgu[:, 0, k, fcl * 128:(fcl + 1) * 128], h2T[:, k, csl],
                                                              start=(k == 0), stop=(k == DC - 1))) for k in range(DC)],
                             reads=[wgu.buf, h2T.buf], writes=[pg.buf])
                        pu = gu_ring.next()
                        S.mm([(lambda e, k=k, pu=pu: e.matmul(pu[:, :], wgu[:, 1, k, fcl * 128:(fcl + 1) * 128], h2T[:, k, csl],
                                                              start=(k == 0), stop=(k == DC - 1))) for k in range(DC)],
                             reads=[wgu.buf, h2T.buf], writes=[pu.buf])
                        sg = sg_ring.next()
                        S.op("act", lambda e, sg=sg, pg=pg: e.activation(out=sg[:], in_=pg[:, :], func=AF.Silu),
                             reads=[pg.buf], writes=[sg.buf])
                        if shared:
                            S.op("dve", lambda e, sg=sg, pu=pu, fc=fc, csl=csl: e.tensor_tensor(
                                out=hidT[:, fc, csl], in0=pu[:, :], in1=sg[:], op=ALU.mult),
                                 reads=[pu.buf, sg.buf], writes=[hidT.buf])
                        else:
                            tt = tt_ring.next()
                            S.op("dve", lambda e, sg=sg, pu=pu, tt=tt: e.tensor_tensor(out=tt[:], in0=pu[:, :], in1=sg[:],
                                                                                       op=ALU.mult),
                                 reads=[pu.buf, sg.buf], writes=[tt.buf])
                            S.op("dve", lambda e, tt=tt, fc=fc, csl=csl: e.tensor_tensor(
                                out=hidT[:, fc, csl], in0=tt[:], in1=wbs[:, csl], op=ALU.mult),
                                 reads=[tt.buf, wbs.buf], writes=[hidT.buf])
            return hidT, wd_t

        def down_phase(hidT, wd_t):
            for i in range(NT):
                tsl = slice(i * 128, (i + 1) * 128)
                for nb in range(4):
                    nsl = slice(nb * 512, (nb + 1) * 512)
                    po = out_ring.next()
                    S.mm([(lambda e, fc=fc, po=po: e.matmul(po[:, :], hidT[:, fc, tsl], wd_t[:, fc, nsl],
                                                            start=(fc == 0), stop=(fc == 3))) for fc in range(4)],
                         reads=[hidT.buf, wd_t.buf], writes=[po.buf])
                    S.op("dve", lambda e, po=po, i=i, nsl=nsl: e.tensor_tensor(out=acc[:, i, nsl], in0=acc[:, i, nsl],
                                                                               in1=po[:, :], op=ALU.add),
                         reads=[accb[i][nb], po.buf], writes=[accb[i][nb]])

        prev = gu_phase(elist[0])
        for e_ in elist[1:]:
            cur = gu_phase(e_)
            down_phase(*prev)
            prev = cur
        down_phase(*prev)
        A.free(wbs, h2T, wT, *hid_ring.items, *wgu_ring.items, *wd_ring.items, *sg_ring.items, *tt_ring.items)
        for i in range(NT):
            tsl = slice(i * 128, (i + 1) * 128)
            x1t = A.alloc("x1t", [D], F32)
            S.dma("sp", x1t[:], y[tsl, :], reads=[ybufs[i]], writes=[x1t.buf])
            S.op("dve", lambda e: e.tensor_tensor(out=acc[:, i, :], in0=acc[:, i, :], in1=gt2b[:], op=ALU.mult),
                 reads=accb[i] + [gt2b.buf], writes=accb[i])
            S.op("dve", lambda e: e.tensor_tensor(out=x1t[:], in0=x1t[:], in1=acc[:, i, :], op=ALU.add),
                 reads=accb[i] + [x1t.buf], writes=[x1t.buf])
            S.dma("sp", y[tsl, :], x1t[:], reads=[x1t.buf], writes=[ybufs[i]])
            A.free(x1t)
        S.finish("sp", ybufs)
        finish_all()
        print("program built: ninst=%d sbuf_peak=%d" % (S.ninst, A.peak))
        return nc, declared, list(dbg_outs.keys())


def rope_tables(pos):
    half = 32
    freqs = (10000.0 ** (-np.arange(half, dtype=np.float32) / half)).astype(np.float32)
    ang = pos.astype(np.float32)[:, None] * freqs[None, :]
    return np.cos(ang).astype(np.float32), np.sin(ang).astype(np.float32)


def core_constants(hf):
    cs = {}
    slot = np.arange(SLOTS)
    pos = (slot - 1024 * (1 - hf)).astype(np.float32)
    c, s = rope_tables(pos)
    cs["cos_s"] = np.ascontiguousarray(c.reshape(NS, 128, 32).transpose(1, 0, 2))
    cs["sin_s"] = np.ascontiguousarray(s.reshape(NS, 128, 32).transpose(1, 0, 2))
    n = np.arange(128)
    n_real = n - 64 * (1 - hf)
    c, s = rope_tables(16.0 * n_real + 15.5)
    cs["cos_c"], cs["sin_c"] = c, s
    t_real = hf * 1024 + np.arange(TOK)
    vis = (n_real[:, None] >= 0) & (16 * n_real[:, None] + 31 <= t_real[None, :]) & (n[:, None] < 127)
    cs["cmpmask"] = vis.astype(np.float32)
    j = np.arange(32)
    ov = (16 * n[:, None] <= 64 * j[None, :] + 63) & (16 * n[:, None] + 31 >= 64 * j[None, :]) & (n[:, None] < 127)
    cs["overlap"] = ov.astype(np.float32)
    j_real = j - 16 * (1 - hf)
    cur = t_real // 64
    visible = (j_real[None, :] >= 0) & (j_real[None, :] <= cur[:, None])
    forced = visible & ((j_real[None, :] == 0) | (j_real[None, :] == cur[:, None]) | (j_real[None, :] == cur[:, None] - 1))
    cstv = np.where(visible, np.where(forced, 100.0, 0.0), -1.0).astype(np.float32)
    cs["cst"] = np.ascontiguousarray(cstv.reshape(NT, 128, 32).transpose(1, 0, 2))
    em = np.zeros((32, NS, 128), np.float32)
    for kt in range(NS):
        for p in range(128):
            em[2 * kt + p // 64, kt, p] = BIG
    cs["emat"] = em
    p = np.arange(128)
    cs["dmask"] = (p[:, None] <= p[None, :]).astype(np.float32)
    cs["lmask"] = (p[:, None] > p[None, :]).astype(np.float32)
    sv = (pos >= 0).astype(np.float32)
    cs["svalid"] = np.ascontiguousarray(sv.reshape(NS, 128).T)
    return cs


def prep_inputs(inp):
    f = lambda a: np.ascontiguousarray(np.asarray(a, dtype=np.float32))
    x = f(inp["x"])
    c = f(inp["c"])
    shared = {
        "w_ada": f(inp["w_ada"][0]), "b_ada": f(inp["b_ada"][0][None]),
        "gn1": f(np.asarray(inp["g_norm1"][0]).reshape(DC, 128).T),
        "gn2": f(np.asarray(inp["g_norm2"][0]).reshape(DC, 128).T),
        "gout": f(np.concatenate([np.asarray(inp["g_out_nsa"][0]), np.asarray(inp["g_out_gmlp"][0])]).reshape(DC, 128).T),
        "w_in": f(inp["w_in"][0]),
        "g_q": f(inp["g_q"][0][None]), "g_k": f(inp["g_k"][0][None]),
        "posk": f(np.asarray(inp["cmp_pos_k"][0]).T), "posv": f(np.asarray(inp["cmp_pos_v"][0]).T),
        "w1k": f(inp["cmp_w1_k"][0]), "w1v": f(inp["cmp_w1_v"][0]),
        "w2k": f(inp["cmp_w2_k"][0]), "w2v": f(inp["cmp_w2_v"][0]),
        "g_gv": f(np.asarray(inp["g_gmlp_v"][0]).reshape(1, 1024)),
        "wspT": f(np.asarray(inp["w_spatial"][0]).transpose(0, 2, 1)),
        "bsp": f(np.asarray(inp["b_spatial"][0]).T),
        "w_out": f(inp["w_out"][0]), "w_r": f(inp["w_router"][0]), "r_bias": f(inp["router_bias"][0][None]),
        "w_gate": f(inp["w_gate"][0]), "w_up": f(inp["w_up"][0]), "w_down": f(inp["w_down"][0]),
        "ws_gate": f(inp["ws_gate"][0]), "ws_up": f(inp["ws_up"][0]), "ws_down": f(inp["ws_down"][0]),
    }
    consts = [core_constants(0), core_constants(1)]
    maps = []
    for core in range(8):
        b, hf = core // 2, core % 2
        m = dict(shared)
        if hf == 1:
            m["xkv"] = f(x[b])
        else:
            m["xkv"] = f(np.concatenate([np.zeros((TOK, D), np.float32), x[b, :TOK]], axis=0))
        m["ct"] = f(c[b].reshape(DC, 128).T)
        m.update(consts[hf])
        maps.append(m)
    return maps


def kernel(**inputs):
    nc, declared, _ = build_program()
    maps = prep_inputs(inputs)
    in_maps = [{k: m[k] for k in declared} for m in maps]
    res = run_bass_kernel_spmd(nc, in_maps, core_ids=list(range(8)))
    out = np.zeros((4, 2048, D), np.float32)
    for core in range(8):
        b, hf = core // 2, core % 2
        out[b, hf * TOK:(hf + 1) * TOK] = res.results[core]["y"]
    return out
```

```python
import os
import numpy as np
import ml_dtypes
from contextlib import ExitStack
import concourse.bass as bass
import concourse.mybir as mybir
from concourse.bass_utils import run_bass_kernel_spmd

F32 = mybir.dt.float32
BF16 = mybir.dt.bfloat16
AF = mybir.ActivationFunctionType
ALU = mybir.AluOpType
AX = mybir.AxisListType

D = 2048
DC = 16
NT = 8
NS = 16
TOK = 1024
SLOTS = 2048
NH = 16
NKV = 4
HD = 64
NE = 64
EPS = 1e-6
BIG = 30000.0
IN_COLS = 4656
GORDER = [0, 2, 1, 3]


class Buf:
    def __init__(self, name):
        self.name = name
        self.w = None
        self.r = []


class Sched:
    def __init__(self, nc, es, n_dma_sems=32):
        self.nc = nc
        self.eng = {"pe": nc.tensor, "act": nc.scalar, "dve": nc.vector, "pool": nc.gpsimd, "sp": nc.sync}
        self.sem = {}
        self.cnt = {}
        for k in self.eng:
            self.sem[k] = es.enter_context(nc.semaphore("s_" + k))
            self.cnt[k] = 0
        self.dsem = [es.enter_context(nc.semaphore("d%d" % i)) for i in range(n_dma_sems)]
        self.dcnt = [0] * n_dma_sems
        self.dnext = 0
        self.known = {k: {} for k in self.eng}
        self.ninst = 0

    def _semobj(self, key):
        return self.sem[key] if isinstance(key, str) else self.dsem[key]

    def _wait(self, e, key, val):
        if key == e and e == "pe":
            return
        kn = self.known[e]
        if kn.get(key, 0) >= val:
            return
        self.eng[e].wait_ge(self._semobj(key), val)
        kn[key] = val

    def _deps(self, e, reads, writes):
        for b in reads:
            if b.w is not None:
                self._wait(e, *b.w)
        for b in writes:
            if b.w is not None:
                self._wait(e, *b.w)
            for r in b.r:
                self._wait(e, *r)

    def _mark(self, tok, reads, writes):
        for b in reads:
            b.r.append(tok)
            if len(b.r) > 24:
                b.r = b.r[-24:] if False else self._compress(b.r)
        for b in writes:
            b.w = tok
            b.r = []

    @staticmethod
    def _compress(toks):
        best = {}
        for k, v in toks:
            if best.get(k, 0) < v:
                best[k] = v
        return list(best.items())

    def op(self, e, fn, reads=(), writes=()):
        self._deps(e, reads, writes)
        inst = fn(self.eng[e])
        self.cnt[e] += 1
        inst.then_inc(self.sem[e], 1)
        self.ninst += 1
        self._mark((e, self.cnt[e]), reads, writes)
        return inst

    def mm(self, fns, reads=(), writes=()):
        e = "pe"
        self._deps(e, reads, writes)
        inst = None
        for fn in fns:
            inst = fn(self.eng[e])
            self.ninst += 1
        self.cnt[e] += 1
        inst.then_inc(self.sem[e], 1)
        self._mark((e, self.cnt[e]), reads, writes)

    def dma(self, e, out, in_, reads=(), writes=(), **kw):
        self._deps(e, reads, writes)
        i = self.dnext
        self.dnext = (self.dnext + 1) % len(self.dsem)
        if self.dcnt[i] > 0:
            self._wait(e, i, self.dcnt[i])
        inst = self.eng[e].dma_start(out=out, in_=in_, **kw)
        self.dcnt[i] += 16
        inst.then_inc(self.dsem[i], 16)
        self.ninst += 1
        self._mark((i, self.dcnt[i]), reads, writes)

    def finish(self, e, bufs):
        for b in bufs:
            if b.w is not None:
                self._wait(e, *b.w)


class Tile:
    def __init__(self, ap, buf, off, nbytes):
        self.ap = ap
        self.buf = buf
        self.off = off
        self.nbytes = nbytes

    def __getitem__(self, idx):
        return self.ap[idx]


class Arena:
    def __init__(self, nc, es, kb=206):
        self.total = kb * 1024
        self.t = es.enter_context(nc.sbuf_tensor("arena", [128, self.total // 4], F32))
        self.used = []
        self.ghosts = []
        self.peak = 0

    def alloc(self, name, shape, dtype, parts=128):
        esz = 4 if dtype == F32 else 2
        n = 1
        for s in shape:
            n *= s
        nbytes = (n * esz + 63) // 64 * 64
        off = 0
        for (o, e_, _) in sorted(self.used, key=lambda u: u[0]):
            if off + nbytes <= o:
                break
            off = max(off, e_)
        if off + nbytes > self.total:
            raise RuntimeError("SBUF arena OOM for %s (%d B); used=%s" % (
                name, nbytes, [(u[2].buf.name, u[1] - u[0]) for u in self.used]))
        ap = self.t[0:parts, off // 4:(off + nbytes) // 4]
        if dtype != F32:
            ap = ap.bitcast(dtype)
        ap = ap[:, 0:n]
        if len(shape) == 2:
            ap = ap.rearrange("p (a b) -> p a b", b=shape[1])
        elif len(shape) == 3:
            ap = ap.rearrange("p (a b c) -> p a b c", b=shape[1], c=shape[2])
        buf = Buf(name)
        toks = []
        keep = []
        for (o, e_, tk) in self.ghosts:
            if o < off + nbytes and off < e_:
                toks.extend(tk)
                if o < off:
                    keep.append((o, off, tk))
                if e_ > off + nbytes:
                    keep.append((off + nbytes, e_, tk))
            else:
                keep.append((o, e_, tk))
        self.ghosts = keep
        buf.r = Sched._compress(toks)
        t = Tile(ap, buf, off, nbytes)
        self.used.append((off, off + nbytes, t))
        self.peak = max(self.peak, max(u[1] for u in self.used))
        return t

    def free(self, *tiles):
        for t in tiles:
            self.used = [u for u in self.used if u[2] is not t]
            toks = list(t.buf.r)
            if t.buf.w is not None:
                toks.append(t.buf.w)
            self.ghosts.append((t.off, t.off + t.nbytes, Sched._compress(toks)))


class Ring:
    def __init__(self, items):
        self.items = items
        self.i = 0

    def next(self):
        it = self.items[self.i]
        self.i = (self.i + 1) % len(self.items)
        return it


def build_program(stop=None, dbg=()):
    nc = bass.Bass("TRN2", target_bir_lowering=False)

    declared = []

    def din(name, shape, dt=F32):
        declared.append(name)
        return nc.dram_tensor(name, list(shape), dt, kind="ExternalInput").ap()

    xkv = din("xkv", [SLOTS, D])
    ct = din("ct", [128, DC])
    w_ada = din("w_ada", [D, 6 * D])
    b_ada = din("b_ada", [1, 6 * D])
    gn1 = din("gn1", [128, DC])
    gn2 = din("gn2", [128, DC])
    gout = din("gout", [128, DC])
    w_in = din("w_in", [D, IN_COLS])
    g_q = din("g_q", [1, HD])
    g_k = din("g_k", [1, HD])
    posk = din("posk", [HD, 32])
    posv = din("posv", [HD, 32])
    w1k = din("w1k", [2048, 256])
    w1v = din("w1v", [2048, 256])
    w2k = din("w2k", [256, HD])
    w2v = din("w2v", [256, HD])
    g_gv = din("g_gv", [1, 1024])
    wspT = din("wspT", [8, 128, 128])
    bsp = din("bsp", [128, 8])
    w_out = din("w_out", [D, D])
    w_r = din("w_r", [D, NE])
    r_bias = din("r_bias", [1, NE])
    cos_s = din("cos_s", [128, NS, 32])
    sin_s = din("sin_s", [128, NS, 32])
    cos_c = din("cos_c", [128, 32])
    sin_c = din("sin_c", [128, 32])
    cmpmask = din("cmpmask", [128, TOK])
    overlap = din("overlap", [128, 32])
    cst = din("cst", [128, NT, 32])
    emat = din("emat", [32, NS, 128])
    dmask = din("dmask", [128, 128])
    lmask = din("lmask", [128, 128])
    svalid = din("svalid", [128, NS])

    y = nc.dram_tensor("y", [TOK, D], F32, kind="ExternalOutput").ap()
    dbg_outs = {}

    with ExitStack() as es:
        S = Sched(nc, es)
        A = Arena(nc, es)
        psall = es.enter_context(nc.psum_tensor("psall", [128, 4096], F32))
        banks = [Tile(psall[:, i * 512:(i + 1) * 512], Buf("psb%d" % i), 0, 0) for i in range(8)]

        def pair_view(s_):
            return psall[:, (2 * s_) * 512:(2 * s_ + 2) * 512].rearrange("p (b c) -> p b c", c=512)[:, :, 0:256]

        def dbg_dump(name, tile_ap, shape, dt, reads):
            o = nc.dram_tensor("dbg_" + name, list(shape), dt, kind="ExternalOutput").ap()
            b = Buf("dbg_" + name)
            S.dma("sp", o, tile_ap, reads=reads, writes=[b])
            dbg_outs[name] = b

        def finish_all():
            S.finish("sp", list(dbg_outs.values()) + [ybuf])
            for e in ("pe", "act", "dve", "pool"):
                S._wait("sp", e, S.cnt[e])
            for i in range(len(S.dsem)):
                if S.dcnt[i]:
                    S._wait("sp", i, S.dcnt[i])

        ybuf = Buf("y")

        ident = A.alloc("ident", [128], F32)
        S.op("pool", lambda e: e.memset(ident[:], 1.0), writes=[ident.buf])
        S.op("pool", lambda e: e.affine_select(out=ident[:], in_=ident[:], pattern=[[1, 128]],
                                               compare_op=ALU.is_equal, fill=0.0, base=0,
                                               channel_multiplier=-1),
             reads=[ident.buf], writes=[ident.buf])
        identb = A.alloc("identb", [128], BF16)
        S.op("dve", lambda e: e.tensor_copy(out=identb[:], in_=ident[:]), reads=[ident.buf], writes=[identb.buf])
        ones = A.alloc("ones", [128], F32)
        S.op("pool", lambda e: e.memset(ones[:], 1.0), writes=[ones.buf])

        modT = A.alloc("modT", [96], F32)
        gt1b = A.alloc("gt1b", [D], F32)
        gt2b = A.alloc("gt2b", [D], F32)
        gsc1 = A.alloc("gsc1", [DC], F32)
        gsc2 = A.alloc("gsc2", [DC], F32)
        g1t = A.alloc("g1t", [DC], F32)
        g2t = A.alloc("g2t", [DC], F32)
        S.dma("sp", g1t[:], gn1[:, :], writes=[g1t.buf])
        S.dma("sp", g2t[:], gn2[:, :], writes=[g2t.buf])

        ctt = A.alloc("ctt", [DC], F32)
        scb = A.alloc("scb", [DC], BF16)
        S.dma("sp", ctt[:], ct[:, :], writes=[ctt.buf])
        S.op("act", lambda e: e.activation(out=scb[:], in_=ctt[:], func=AF.Silu), reads=[ctt.buf], writes=[scb.buf])
        modrow = A.alloc("modrow", [6 * D], F32, parts=1)
        brow = A.alloc("brow", [6 * D], F32, parts=1)
        S.dma("sp", brow[:], b_ada[0:1, :], writes=[brow.buf])
        wa_ring = Ring([A.alloc("wa%d" % i, [DC, 512], BF16) for i in range(3)])
        for cb in range(24):
            wa = wa_ring.next()
            S.dma("pool", wa[:], w_ada[:, cb * 512:(cb + 1) * 512].rearrange("(c p) f -> p c f", p=128),
                  writes=[wa.buf])
            ps = banks[cb % 2]
            S.mm([(lambda e, k=k, wa=wa, ps=ps: e.matmul(ps[0:1, :], scb[:, k:k + 1], wa[:, k, :],
                                                        start=(k == 0), stop=(k == DC - 1)))
                  for k in range(DC)], reads=[scb.buf, wa.buf], writes=[ps.buf])
            S.op("dve", lambda e, ps=ps, cb=cb: e.tensor_tensor(out=modrow[:, cb * 512:(cb + 1) * 512],
                                                               in0=ps[0:1, :], in1=brow[:, cb * 512:(cb + 1) * 512],
                                                               op=ALU.add),
                 reads=[ps.buf, brow.buf], writes=[modrow.buf])
        ps = banks[2]
        S.mm([(lambda e, j=j: e.matmul(ps[:, j:j + 1], modrow[0:1, j * 128:(j + 1) * 128], ones[0:1, 0:1],
                                       start=True, stop=True)) for j in range(96)],
             reads=[modrow.buf, ones.buf], writes=[ps.buf])
        S.op("dve", lambda e: e.tensor_copy(out=modT[:], in_=ps[:, 0:96]), reads=[ps.buf], writes=[modT.buf])
        S.op("dve", lambda e: e.scalar_tensor_tensor(out=gsc1[:], in0=modT[:, 16:32], scalar=1.0, in1=g1t[:],
                                                     op0=ALU.add, op1=ALU.mult),
             reads=[modT.buf, g1t.buf], writes=[gsc1.buf])
        S.op("dve", lambda e: e.scalar_tensor_tensor(out=gsc2[:], in0=modT[:, 64:80], scalar=1.0, in1=g2t[:],
                                                     op0=ALU.add, op1=ALU.mult),
             reads=[modT.buf, g2t.buf], writes=[gsc2.buf])
        for (dst, base) in ((gt1b, 2 * D), (gt2b, 5 * D)):
            for j in range(4):
                ps = banks[3 + (j % 2)]
                S.mm([lambda e, ps=ps, base=base, j=j: e.matmul(ps[:, :], ones[0:1, :],
                                                               modrow[0:1, base + j * 512: base + (j + 1) * 512],
                                                               start=True, stop=True)],
                     reads=[modrow.buf, ones.buf], writes=[ps.buf])
                S.op("act", lambda e, ps=ps, dst=dst, j=j: e.activation(out=dst[:, j * 512:(j + 1) * 512], in_=ps[:, :],
                                                                       func=AF.Copy),
                     reads=[ps.buf], writes=[dst.buf])
        A.free(modrow, brow, ctt, scb, g1t, g2t, *wa_ring.items)
        if "mod" in dbg:
            dbg_dump("modT", modT[:], [128, 96], F32, [modT.buf])
            dbg_dump("gt1b", gt1b[:], [128, D], F32, [gt1b.buf])
        if stop == "ada":
            finish_all()
            return nc, declared, list(dbg_outs.keys())

        hT = [A.alloc("hT_prev", [DC, TOK], BF16), A.alloc("hT_own", [DC, TOK], BF16)]

        def norm_transpose(xt, xbuf, gsc, shcol0, dst, dst_buf, col0, f32dst=None):
            sq = A.alloc("sq", [D], F32)
            ss = A.alloc("ss", [4], F32)
            S.op("act", lambda e: e.activation(out=sq[:], in_=xt, func=AF.Square), reads=[xbuf], writes=[sq.buf])
            S.op("dve", lambda e: e.reduce_sum(out=ss[:, 0:1], in_=sq[:], axis=AX.X), reads=[sq.buf], writes=[ss.buf])
            S.op("dve", lambda e: e.tensor_scalar(out=ss[:, 1:2], in0=ss[:, 0:1], scalar1=1.0 / D, scalar2=EPS,
                                                  op0=ALU.mult, op1=ALU.add), reads=[ss.buf], writes=[ss.buf])
            S.op("act", lambda e: e.sqrt(out=ss[:, 2:3], in_=ss[:, 1:2]), reads=[ss.buf], writes=[ss.buf])
            S.op("dve", lambda e: e.reciprocal(out=ss[:, 3:4], in_=ss[:, 2:3]), reads=[ss.buf], writes=[ss.buf])
            S.op("dve", lambda e: e.tensor_scalar(out=sq[:], in0=xt, scalar1=ss[:, 3:4], scalar2=None, op0=ALU.mult),
                 reads=[xbuf, ss.buf], writes=[sq.buf])
            for c4 in range(4):
                ps = psring.next()
                S.mm([(lambda e, c=c, ps=ps: e.transpose(ps[:, (c % 4) * 128:(c % 4 + 1) * 128],
                                                         sq[:, c * 128:(c + 1) * 128], ident[:]))
                      for c in range(c4 * 4, c4 * 4 + 4)], reads=[sq.buf, ident.buf], writes=[ps.buf])
                for c in range(c4 * 4, c4 * 4 + 4):
                    tgt = dst[:, c, col0:col0 + 128] if f32dst is None else f32dst[:, c, :]
                    S.op("act", lambda e, c=c, ps=ps, tgt=tgt: e.activation(
                        out=tgt, in_=ps[:, (c % 4) * 128:(c % 4 + 1) * 128], func=AF.Identity,
                        scale=gsc[:, c:c + 1], bias=modT[:, shcol0 + c: shcol0 + c + 1]),
                         reads=[ps.buf, gsc.buf, modT.buf],
                         writes=[dst_buf if f32dst is None else f32dst.buf])
            A.free(sq, ss)

        psring = Ring(banks[0:8])
        xring = Ring([A.alloc("xt%d" % i, [D], F32) for i in range(3)])
        for st in range(NS):
            xt = xring.next()
            S.dma("sp", xt[:], xkv[st * 128:(st + 1) * 128, :], writes=[xt.buf])
            norm_transpose(xt[:], xt.buf, gsc1, 0, hT[st // 8], hT[st // 8].buf, (st % 8) * 128)
        A.free(*xring.items)
        if "hT" in dbg:
            dbg_dump("hT_prev", hT[0][:], [128, DC, TOK], BF16, [hT[0].buf])
            dbg_dump("hT_own", hT[1][:], [128, DC, TOK], BF16, [hT[1].buf])
        if stop == "norm1":
            finish_all()
            return nc, declared, list(dbg_outs.keys())

        def bc_load(name, src, n):
            t = A.alloc(name, [n], F32)
            S.dma("sp", t[:], src[0:1, :].to_broadcast([128, n]), writes=[t.buf])
            return t

        gqb = bc_load("gqb", g_q, HD)
        gkb = bc_load("gkb", g_k, HD)
        ggvb = bc_load("ggvb", g_gv, 1024)
        negM = A.alloc("negM", [4], F32)
        S.op("dve", lambda e: e.reduce_max(out=negM[:, 0:1], in_=gqb[:], axis=AX.X, apply_absolute_value=True),
             reads=[gqb.buf], writes=[negM.buf])
        S.op("dve", lambda e: e.reduce_max(out=negM[:, 1:2], in_=gkb[:], axis=AX.X, apply_absolute_value=True),
             reads=[gkb.buf], writes=[negM.buf])
        S.op("dve", lambda e: e.tensor_tensor(out=negM[:, 2:3], in0=negM[:, 0:1], in1=negM[:, 1:2], op=ALU.mult),
             reads=[negM.buf], writes=[negM.buf])
        S.op("dve", lambda e: e.tensor_scalar(out=negM[:, 3:4], in0=negM[:, 2:3], scalar1=-8.0, scalar2=None, op0=ALU.mult),
             reads=[negM.buf], writes=[negM.buf])
        coss = A.alloc("coss", [NS, 32], F32)
        sins = A.alloc("sins", [NS, 32], F32)
        S.dma("sp", coss[:], cos_s[:, :, :], writes=[coss.buf])
        S.dma("sp", sins[:], sin_s[:, :, :], writes=[sins.buf])
        svt = A.alloc("svt", [NS], F32)
        S.dma("sp", svt[:], svalid[:, :], writes=[svt.buf])
        dmt = A.alloc("dmt", [128], F32)
        S.dma("sp", dmt[:], dmask[:, :], writes=[dmt.buf])
        goutt = A.alloc("goutt", [DC], F32)
        S.dma("sp", goutt[:], gout[:, :], writes=[goutt.buf])
        bspt = A.alloc("bspt", [8], F32)
        S.dma("sp", bspt[:], bsp[:, :], writes=[bspt.buf])

        wring = Ring([A.alloc("wblk%d" % i, [DC, 512], BF16) for i in range(2)])

        def load_wblk(c0, ncols):
            wb = wring.next()
            S.dma("pool", wb[:, :, 0:ncols], w_in[:, c0:c0 + ncols].rearrange("(c p) f -> p c f", p=128),
                  writes=[wb.buf])
            return wb

        def proj(wb, ncols, st):
            ps = psring.next()
            src = hT[st // 8]
            col0 = (st % 8) * 128
            S.mm([(lambda e, k=k: e.matmul(ps[:, 0:ncols], src[:, k, col0:col0 + 128], wb[:, k, 0:ncols],
                                           start=(k == 0), stop=(k == DC - 1))) for k in range(DC)],
                 reads=[src.buf, wb.buf], writes=[ps.buf])
            return ps

        def rstd_from_ss(st_, n, inv_n):
            S.op("dve", lambda e: e.tensor_scalar(out=st_[:, 1, :], in0=st_[:, 0, :], scalar1=inv_n, scalar2=EPS,
                                                  op0=ALU.mult, op1=ALU.add), reads=[st_.buf], writes=[st_.buf])
            S.op("act", lambda e: e.sqrt(out=st_[:, 2, :], in_=st_[:, 1, :]), reads=[st_.buf], writes=[st_.buf])
            S.op("dve", lambda e: e.reciprocal(out=st_[:, 3, :], in_=st_[:, 2, :]), reads=[st_.buf], writes=[st_.buf])

        def norm_rope(src_ap, src_buf, nh, gb, cos_ap, sin_ap, tabs, out_ap, out_buf):
            sq = A.alloc("nr_sq", [nh, HD], F32)
            xn = A.alloc("nr_xn", [nh, HD], F32)
            st_ = A.alloc("nr_st", [4, nh], F32)
            src3 = src_ap.rearrange("p (h d) -> p h d", d=HD)
            S.op("act", lambda e: e.activation(out=sq[:], in_=src3, func=AF.Square), reads=[src_buf], writes=[sq.buf])
            S.op("dve", lambda e: e.reduce_sum(out=st_[:, 0, :], in_=sq[:], axis=AX.X), reads=[sq.buf], writes=[st_.buf])
            rstd_from_ss(st_, nh, 1.0 / HD)
            S.op("dve", lambda e: e.tensor_tensor(out=xn[:], in0=src3,
                                                  in1=st_[:, 3, :].unsqueeze(2).to_broadcast([128, nh, HD]),
                                                  op=ALU.mult), reads=[src_buf, st_.buf], writes=[xn.buf])
            S.op("dve", lambda e: e.tensor_tensor(out=xn[:], in0=xn[:],
                                                  in1=gb[:].unsqueeze(1).to_broadcast([128, nh, HD]),
                                                  op=ALU.mult), reads=[xn.buf, gb.buf], writes=[xn.buf])
            cb = cos_ap.unsqueeze(1).to_broadcast([128, nh, 32])
            sb = sin_ap.unsqueeze(1).to_broadcast([128, nh, 32])
            x1 = xn[:, :, 0:32]
            x2 = xn[:, :, 32:64]
            S.op("dve", lambda e: e.tensor_tensor(out=sq[:, :, 0:32], in0=x1, in1=cb, op=ALU.mult),
                 reads=[xn.buf] + tabs, writes=[sq.buf])
            S.op("dve", lambda e: e.tensor_tensor(out=sq[:, :, 32:64], in0=x2, in1=sb, op=ALU.mult),
                 reads=[xn.buf] + tabs, writes=[sq.buf])
            S.op("dve", lambda e: e.tensor_tensor(out=out_ap[:, :, 0:32], in0=sq[:, :, 0:32], in1=sq[:, :, 32:64],
                                                  op=ALU.subtract), reads=[sq.buf], writes=[out_buf])
            S.op("dve", lambda e: e.tensor_tensor(out=sq[:, :, 0:32], in0=x2, in1=cb, op=ALU.mult),
                 reads=[xn.buf] + tabs, writes=[sq.buf])
            S.op("dve", lambda e: e.tensor_tensor(out=sq[:, :, 32:64], in0=x1, in1=sb, op=ALU.mult),
                 reads=[xn.buf] + tabs, writes=[sq.buf])
            S.op("dve", lambda e: e.tensor_tensor(out=out_ap[:, :, 32:64], in0=sq[:, :, 0:32], in1=sq[:, :, 32:64],
                                                  op=ALU.add), reads=[sq.buf], writes=[out_buf])
            A.free(sq, xn, st_)

        def bank_bf(ps):
            return ps[:, :].bitcast(BF16)

        kcT = A.alloc("kcT", [NKV, SLOTS], BF16)
        vcT = A.alloc("vcT", [NKV, SLOTS], BF16)
        wb = load_wblk(1024, 512)
        for st in range(NS):
            ps = proj(wb, 512, st)
            raw = A.alloc("raw", [512], BF16)
            S.op("act", lambda e: e.activation(out=raw[:], in_=ps[:, :], func=AF.Copy), reads=[ps.buf], writes=[raw.buf])
            pt = psring.next()
            ptb = bank_bf(pt)
            S.mm([(lambda e, j=j: e.transpose(ptb[0:64, j * 128:(j + 1) * 128], raw[:, j * 64:(j + 1) * 64], identb[:]))
                  for j in range(8)], reads=[raw.buf, identb.buf], writes=[pt.buf])
            S.op("dve", lambda e: e.tensor_copy(out=kcT[0:64, :, st * 128:(st + 1) * 128],
                                                in_=ptb[0:64, 0:512].rearrange("p (h t) -> p h t", t=128)),
                 reads=[pt.buf], writes=[kcT.buf])
            S.op("dve", lambda e: e.tensor_copy(out=vcT[0:64, :, st * 128:(st + 1) * 128],
                                                in_=ptb[0:64, 512:1024].rearrange("p (h t) -> p h t", t=128)),
                 reads=[pt.buf], writes=[vcT.buf])
            A.free(raw)
        if "kc" in dbg:
            dbg_dump("kcT", kcT[0:64], [64, NKV, SLOTS], BF16, [kcT.buf])
            dbg_dump("vcT", vcT[0:64], [64, NKV, SLOTS], BF16, [vcT.buf])

        kcmpT2 = A.alloc("kcmpT2", [NKV, 128], BF16)
        vcA = A.alloc("vcA", [NKV, 97], BF16)
        cosc = A.alloc("cosc", [32], F32)
        sinc = A.alloc("sinc", [32], F32)
        ovl = A.alloc("ovl", [32], F32)
        S.dma("sp", cosc[:], cos_c[:, :], writes=[cosc.buf])
        S.dma("sp", sinc[:], sin_c[:, :], writes=[sinc.buf])
        S.dma("sp", ovl[:], overlap[:, :], writes=[ovl.buf])
        S.op("pool", lambda e: e.memset(vcA[:, :, 64:65], 1.0), writes=[vcA.buf])
        S.op("pool", lambda e: e.tensor_copy(out=vcA[:, :, 65:97], in_=ovl[:].unsqueeze(1).to_broadcast([128, NKV, 32])),
             reads=[ovl.buf], writes=[vcA.buf])
        for kind in range(2):
            srcT = kcT if kind == 0 else vcT
            w1t = A.alloc("w1t", [32, 256], BF16)
            w2t = A.alloc("w2t", [2, HD], BF16)
            post = A.alloc("post", [32], F32)
            S.dma("pool", w1t[0:64], (w1k if kind == 0 else w1v).rearrange("(l d) h -> d l h", d=HD), writes=[w1t.buf])
            S.dma("pool", w2t[:], (w2k if kind == 0 else w2v).rearrange("(c p) d -> p c d", p=128), writes=[w2t.buf])
            S.dma("sp", post[0:64], (posk if kind == 0 else posv)[:, :], writes=[post.buf])
            for hk in range(NKV):
                blk = A.alloc("blk", [32, 127], BF16)
                gT = A.alloc("gT", [2, 127], BF16)
                src = srcT[0:64, hk, :].rearrange("p (n l) -> p n l", l=16)
                for half in range(2):
                    S.op("dve", lambda e, half=half: e.tensor_tensor(
                        out=blk[0:64, 16 * half:16 * half + 16, :],
                        in0=src[:, half:half + 127, :].rearrange("p n l -> p l n"),
                        in1=post[0:64, 16 * half:16 * half + 16].unsqueeze(2).to_broadcast([64, 16, 127]),
                        op=ALU.add), reads=[srcT.buf, post.buf], writes=[blk.buf])
                for hc in range(2):
                    pg = psring.next()
                    S.mm([(lambda e, l=l, hc=hc, pg=pg: e.matmul(pg[:, 0:127], w1t[0:64, l, hc * 128:(hc + 1) * 128],
                                                               blk[0:64, l, :], start=(l == 0), stop=(l == 31)))
                          for l in range(32)], reads=[w1t.buf, blk.buf], writes=[pg.buf])
                    S.op("act", lambda e, hc=hc, pg=pg: e.activation(out=gT[:, hc, :], in_=pg[:, 0:127],
                                                                    func=AF.Gelu_apprx_tanh),
                         reads=[pg.buf], writes=[gT.buf])
                po = psring.next()
                S.mm([(lambda e, c=c: e.matmul(po[0:127, 0:HD], gT[:, c, :], w2t[:, c, :], start=(c == 0), stop=(c == 1)))
                      for c in range(2)], reads=[gT.buf, w2t.buf], writes=[po.buf])
                if kind == 0:
                    kn2 = A.alloc("kcn2", [1, 2, HD], BF16)
                    norm_rope(po[:, 0:HD], po.buf, 1, gkb, cosc[:], sinc[:], [cosc.buf, sinc.buf], kn2[:, :, 0, :], kn2.buf)
                    S.op("pool", lambda e: e.tensor_copy(out=kn2[:, :, 1, :], in_=kn2[:, :, 0, :]),
                         reads=[kn2.buf], writes=[kn2.buf])
                    pt = psring.next()
                    ptb = bank_bf(pt)
                    S.mm([lambda e: e.transpose(ptb[:, 0:128], kn2[:, 0, :, :].rearrange("p a d -> p (a d)"), identb[:])],
                         reads=[kn2.buf, identb.buf], writes=[pt.buf])
                    S.op("act", lambda e: e.activation(out=kcmpT2[:, hk, :], in_=ptb[:, 0:128], func=AF.Copy),
                         reads=[pt.buf], writes=[kcmpT2.buf])
                    A.free(kn2)
                else:
                    S.op("act", lambda e: e.activation(out=vcA[0:127, hk, 0:HD], in_=po[0:127, 0:HD], func=AF.Copy),
                         reads=[po.buf], writes=[vcA.buf])
                A.free(blk, gT)
            A.free(w1t, w2t, post)
        A.free(kcT, vcT, cosc, sinc, ovl)
        if "cmp" in dbg:
            dbg_dump("kcmpT2", kcmpT2[:], [128, NKV, 128], BF16, [kcmpT2.buf])
            dbg_dump("vcA", vcA[:], [128, NKV, 97], BF16, [vcA.buf])

        ksT2 = A.alloc("ksT2", [NKV, SLOTS], BF16)
        kwT2 = A.alloc("kwT2", [NKV, SLOTS], BF16)
        vsA = A.alloc("vsA", [NS, NKV, 65], BF16)
        vwA = A.alloc("vwA", [NS, NKV, 65], BF16)
        for (c0, kT2, vA) in ((1536, ksT2, vsA), (2048, kwT2, vwA)):
            wb = load_wblk(c0, 512)
            ps_next = proj(wb, 512, 0)
            for st in range(NS):
                ps = ps_next
                kn2 = A.alloc("kn2", [NKV, 2, HD], BF16)
                norm_rope(ps[:, 0:256], ps.buf, NKV, gkb, coss[:, st, :], sins[:, st, :], [coss.buf, sins.buf],
                          kn2[:, :, 0, :], kn2.buf)
                S.op("pool", lambda e: e.tensor_copy(out=kn2[:, :, 1, :], in_=kn2[:, :, 0, :]),
                     reads=[kn2.buf], writes=[kn2.buf])
                S.op("dve", lambda e: e.tensor_scalar(out=vA[:, st, :, 0:64],
                                                      in0=ps[:, 256:512].rearrange("p (h d) -> p h d", d=HD),
                                                      scalar1=svt[:, st:st + 1], scalar2=None, op0=ALU.mult),
                     reads=[ps.buf, svt.buf], writes=[vA.buf])
                S.op("dve", lambda e: e.tensor_copy(out=vA[:, st, :, 64:65],
                                                    in_=svt[:, st:st + 1].unsqueeze(1).to_broadcast([128, NKV, 1])),
                     reads=[svt.buf], writes=[vA.buf])
                if st + 1 < NS:
                    ps_next = proj(wb, 512, st + 1)
                pt = psring.next()
                ptb = bank_bf(pt)
                S.mm([(lambda e, h=h: e.transpose(ptb[:, h * 128:(h + 1) * 128],
                                                  kn2[:, h, :, :].rearrange("p a d -> p (a d)"), identb[:]))
                      for h in range(NKV)], reads=[kn2.buf, identb.buf], writes=[pt.buf])
                S.op("act", lambda e: e.activation(out=kT2[:, :, st * 128:(st + 1) * 128],
                                                   in_=ptb[:, 0:512].rearrange("p (h t) -> p h t", t=128), func=AF.Copy),
                     reads=[pt.buf], writes=[kT2.buf])
                A.free(kn2)
        A.free(hT[0])
        if "ks" in dbg:
            dbg_dump("ksT2", ksT2[:], [128, NKV, SLOTS], BF16, [ksT2.buf])
            dbg_dump("kwT2", kwT2[:], [128, NKV, SLOTS], BF16, [kwT2.buf])
            dbg_dump("vsA", vsA[:], [128, NS, NKV, 65], BF16, [vsA.buf])
            dbg_dump("vwA", vwA[:], [128, NS, NKV, 65], BF16, [vwA.buf])
        if stop == "kv":
            finish_all()
            return nc, declared, list(dbg_outs.keys())

        qT = A.alloc("qT", [8, TOK], BF16)
        for qb in range(2):
            wb = load_wblk(qb * 512, 512)
            ps_next = proj(wb, 512, 8)
            for i in range(NT):
                ps = ps_next
                qn = A.alloc("qn", [8, HD], BF16)
                norm_rope(ps[:, :], ps.buf, 8, gqb, coss[:, 8 + i, :], sins[:, 8 + i, :], [coss.buf, sins.buf],
                          qn[:], qn.buf)
                if i + 1 < NT:
                    ps_next = proj(wb, 512, 9 + i)
                pt = psring.next()
                ptb = bank_bf(pt)
                S.mm([(lambda e, j=j: e.transpose(ptb[:, j * 128:(j + 1) * 128],
                                                  qn[:, 2 * j:2 * j + 2, :].rearrange("p a d -> p (a d)"), identb[:]))
                      for j in range(4)], reads=[qn.buf, identb.buf], writes=[pt.buf])
                S.op("act", lambda e: e.activation(out=qT[:, 4 * qb:4 * qb + 4, i * 128:(i + 1) * 128],
                                                   in_=ptb[:, 0:512].rearrange("p (h t) -> p h t", t=128), func=AF.Copy),
                     reads=[pt.buf], writes=[qT.buf])
                A.free(qn)
        gsig = A.alloc("gsig", [NT, 3, NH], F32)
        wb = load_wblk(2560, 48)
        for i in range(NT):
            ps = proj(wb, 48, 8 + i)
            S.op("act", lambda e: e.activation(out=gsig[:, i, :, :], in_=ps[:, 0:48].rearrange("p (h r) -> p r h", r=3),
                                               func=AF.Sigmoid),
                 reads=[ps.buf], writes=[gsig.buf])
        if "q" in dbg:
            dbg_dump("qT", qT[:], [128, 8, TOK], BF16, [qT.buf])
            dbg_dump("gsig", gsig[:], [128, NT, 3, NH], F32, [gsig.buf])
        if stop == "q":
            finish_all()
            return nc, declared, list(dbg_outs.keys())

        A.free(wring.items[1], coss, sins, gqb, gkb)
        wring = Ring([wring.items[0]])
        catTg = A.alloc("catTg", [8, TOK], BF16)
        ssg = A.alloc("ssg", [NT, 2], F32)
        wspf = A.alloc("wspf", [8, 128], F32)
        wsp = A.alloc("wsp", [8, 128], BF16)
        S.dma("sp", wspf[:], wspT.rearrange("g s t -> s g t"), writes=[wspf.buf])
        S.op("dve", lambda e: e.tensor_tensor(out=wsp[:], in0=wspf[:], in1=dmt[:].unsqueeze(1).to_broadcast([128, 8, 128]),
                                              op=ALU.mult), reads=[wspf.buf, dmt.buf], writes=[wsp.buf])
        gu = A.alloc("gu", [NT, 1024], BF16)
        for ub in range(2):
            wb = load_wblk(2608 + ub * 512, 512)
            for i in range(NT):
                ps = proj(wb, 512, 8 + i)
                S.op("act", lambda e: e.activation(out=gu[:, i, ub * 512:(ub + 1) * 512], in_=ps[:, :],
                                                   func=AF.Gelu_apprx_tanh), reads=[ps.buf], writes=[gu.buf])
        for vb in range(2):
            wb = load_wblk(3632 + vb * 512, 512)
            ps_next = proj(wb, 512, 8)
            for i in range(NT):
                ps = ps_next
                gv = A.alloc("gv", [4, 128], F32)
                sq = A.alloc("gsq", [4, 128], F32)
                st_ = A.alloc("gst", [4, 4], F32)
                vn = A.alloc("vn", [4, 128], BF16)
                og = A.alloc("og", [4, 128], F32)
                S.op("act", lambda e: e.activation(out=gv[:], in_=ps[:, :].rearrange("p (g d) -> p g d", d=128),
                                                   func=AF.Gelu_apprx_tanh), reads=[ps.buf], writes=[gv.buf])
                S.op("act", lambda e: e.activation(out=sq[:], in_=gv[:], func=AF.Square), reads=[gv.buf], writes=[sq.buf])
                S.op("dve", lambda e: e.reduce_sum(out=st_[:, 0, :], in_=sq[:], axis=AX.X), reads=[sq.buf], writes=[st_.buf])
                rstd_from_ss(st_, 4, 1.0 / 128)
                S.op("dve", lambda e: e.tensor_tensor(out=gv[:], in0=gv[:],
                                                      in1=st_[:, 3, :].unsqueeze(2).to_broadcast([128, 4, 128]),
                                                      op=ALU.mult), reads=[gv.buf, st_.buf], writes=[gv.buf])
                S.op("dve", lambda e: e.tensor_tensor(out=vn[:], in0=gv[:],
                                                      in1=ggvb[:, vb * 512:(vb + 1) * 512].rearrange("p (g d) -> p g d", d=128),
                                                      op=ALU.mult), reads=[gv.buf, ggvb.buf], writes=[vn.buf])
                if i + 1 < NT:
                    ps_next = proj(wb, 512, 9 + i)
                p2 = psring.next()
                S.mm([(lambda e, g=g: e.matmul(p2[:, g * 128:(g + 1) * 128], wsp[:, 4 * vb + g, :], vn[:, g, :],
                                               start=True, stop=True)) for g in range(4)],
                     reads=[wsp.buf, vn.buf], writes=[p2.buf])
                for g in range(4):
                    G = 4 * vb + g
                    S.op("dve", lambda e, g=g, G=G: e.scalar_tensor_tensor(
                        out=og[:, g, :], in0=p2[:, g * 128:(g + 1) * 128], scalar=bspt[:, G:G + 1],
                        in1=gu[:, i, G * 128:(G + 1) * 128], op0=ALU.add, op1=ALU.mult),
                         reads=[p2.buf, bspt.buf, gu.buf], writes=[og.buf])
                S.op("act", lambda e: e.activation(out=sq[:], in_=og[:], func=AF.Square), reads=[og.buf], writes=[sq.buf])
                S.op("dve", lambda e: e.reduce_sum(out=ssg[:, i, vb:vb + 1], in_=sq[:].rearrange("p g d -> p (g d)"), axis=AX.X),
                     reads=[sq.buf], writes=[ssg.buf])
                p3 = psring.next()
                S.mm([(lambda e, g=g: e.transpose(p3[:, g * 128:(g + 1) * 128], og[:, g, :], ident[:])) for g in range(4)],
                     reads=[og.buf, ident.buf], writes=[p3.buf])
                for g in range(4):
                    c = 8 + 4 * vb + g
                    S.op("act", lambda e, g=g, c=c: e.activation(out=catTg[:, c - 8, i * 128:(i + 1) * 128],
                                                                 in_=p3[:, g * 128:(g + 1) * 128], func=AF.Copy,
                                                                 scale=goutt[:, c:c + 1]),
                         reads=[p3.buf, goutt.buf], writes=[catTg.buf])
                A.free(gv, sq, st_, vn, og)
        A.free(gu, wspf, wsp, hT[1], ggvb, *wring.items)
        if "gmlp" in dbg:
            dbg_dump("catTg", catTg[:], [128, 8, TOK], BF16, [catTg.buf])
            dbg_dump("ssg", ssg[:], [128, NT, 2], F32, [ssg.buf])
        if stop == "gmlp":
            finish_all()
            return nc, declared, list(dbg_outs.keys())

        cmk = A.alloc("cmk", [TOK], BF16)
        S.dma("pool", cmk[:], cmpmask[:, :], writes=[cmk.buf])
        cstt = A.alloc("cstt", [NT, 32], F32)
        S.dma("sp", cstt[:], cst[:, :, :], writes=[cstt.buf])
        emt = A.alloc("emt", [NS, 128], BF16)
        S.dma("pool", emt[0:32], emat[:, :, :], writes=[emt.buf])
        S.dma("pool", emt[64:96], emat[:, :, :], writes=[emt.buf])
        dmb = A.alloc("dmb", [128], BF16)
        lmb = A.alloc("lmb", [128], BF16)
        S.op("dve", lambda e: e.tensor_copy(out=dmb[:], in_=dmt[:]), reads=[dmt.buf], writes=[dmb.buf])
        S.dma("pool", lmb[:], lmask[:, :], writes=[lmb.buf])
        selT = A.alloc("selT", [NKV, TOK], BF16)
        onsa = A.alloc("onsa", [NT, 1024], F32)
        S.op("pool", lambda e: e.memset(onsa[:], 0.0), writes=[onsa.buf])
        Sring = Ring([(0, banks[0], banks[1]), (1, banks[2], banks[3]), (2, banks[4], banks[5])])
        accring = Ring(banks[6:8])
        PTring = Ring([A.alloc("pt%d" % i, [512], BF16) for i in range(6)])

        def qk_fns(pr, kT2, hk, ksl, kw_, i, bias_kt=None):
            fns = []
            for half, pb in ((0, 0), (1, 64)):
                bank = pr[1 + half]
                if bias_kt is not None:
                    fns.append(lambda e, bank=bank, pb=pb: e.matmul(
                        bank[:, 0:256], emt[pb:pb + 32, bias_kt, :],
                        selT[pb:pb + 32, hk, i * 128:(i + 1) * 128].unsqueeze(1).to_broadcast([32, 2, 128]),
                        start=True, stop=False))
                fns.append(lambda e, bank=bank, pb=pb: e.matmul(
                    bank[0:kw_, 0:256], kT2[pb:pb + 64, hk, ksl],
                    qT[pb:pb + 64, 2 * hk:2 * hk + 2, i * 128:(i + 1) * 128],
                    start=(bias_kt is None), stop=True))
            return fns

        def exp_pair(pr, pt, kp):
            S.op("act", lambda e: e.activation(out=pt[0:kp, :].rearrange("p (b c) -> p b c", c=256),
                                               in_=pair_view(pr[0])[0:kp], func=AF.Exp, scale=0.125,
                                               bias=(negM[0:kp, 3:4] if os.environ.get("STAB", "1") == "1" else 0.0)),
                 reads=[pr[1].buf, pr[2].buf, negM.buf], writes=[pt.buf])

        def evac(acc, hk, i, br, want_imp=False):
            r4 = A.alloc("r4", [4], F32)
            cf = A.alloc("cf", [4], F32)
            accv = acc[:, :].rearrange("p (j w) -> p j w", w=128)
            S.op("dve", lambda e: e.tensor_scalar(out=r4[:], in0=accv[:, :, 64], scalar1=1e-30, scalar2=None, op0=ALU.max),
                 reads=[acc.buf], writes=[r4.buf])
            S.op("dve", lambda e: e.reciprocal(out=r4[:], in_=r4[:]), reads=[r4.buf], writes=[r4.buf])
            S.op("dve", lambda e: e.tensor_tensor(out=cf[:].rearrange("p (b a) -> p b a", a=2),
                                                  in0=r4[:].rearrange("p (a b) -> p b a", b=2),
                                                  in1=gsig[:, i, br, 4 * hk:4 * hk + 4].rearrange("p (b a) -> p b a", a=2),
                                                  op=ALU.mult), reads=[r4.buf, gsig.buf], writes=[cf.buf])
            imp = None
            if want_imp:
                imp = A.alloc("imp", [32], F32)
                for j in range(4):
                    if j == 0:
                        S.op("dve", lambda e: e.tensor_scalar(out=imp[:], in0=acc[:, 65:97], scalar1=r4[:, 0:1], scalar2=None,
                                                              op0=ALU.mult), reads=[acc.buf, r4.buf], writes=[imp.buf])
                    else:
                        S.op("dve", lambda e, j=j: e.scalar_tensor_tensor(out=imp[:], in0=acc[:, j * 128 + 65:j * 128 + 97],
                                                                         scalar=r4[:, j:j + 1], in1=imp[:],
                                                                         op0=ALU.mult, op1=ALU.add),
                             reads=[acc.buf, r4.buf, imp.buf], writes=[imp.buf])
            for j in range(4):
                g = GORDER[j]
                h = 4 * hk + g
                S.op("dve", lambda e, j=j, g=g, h=h: e.scalar_tensor_tensor(
                    out=onsa[:, i, h * 64:(h + 1) * 64], in0=acc[:, j * 128:j * 128 + 64], scalar=cf[:, g:g + 1],
                    in1=onsa[:, i, h * 64:(h + 1) * 64], op0=ALU.mult, op1=ALU.add),
                     reads=[acc.buf, cf.buf, onsa.buf], writes=[onsa.buf])
            A.free(r4, cf)
            return imp

        def pv(acc, pt, kp, vA_ap, vbuf, width, first, last):
            S.mm([(lambda e, j=j: e.matmul(acc[:, j * 128:j * 128 + width], pt[0:kp, j * 128:(j + 1) * 128], vA_ap,
                                           start=(first and j == 0), stop=last, skip_group_check=True)) for j in range(4)],
                 reads=[pt.buf, vbuf], writes=[acc.buf])

        def mask_mul(pt, kp, mk_ap, mbuf):
            S.op("pool", lambda e: e.tensor_tensor(out=pt[0:kp, :].rearrange("p (j q) -> p j q", q=128),
                                                   in0=pt[0:kp, :].rearrange("p (j q) -> p j q", q=128),
                                                   in1=mk_ap.unsqueeze(1).to_broadcast([kp, 4, 128]), op=ALU.mult),
                 reads=[pt.buf, mbuf], writes=[pt.buf])

        units = []
        for hk in range(NKV):
            for i in range(NT):
                units.append(dict(kind="cmp", hk=hk, i=i, kt=0, first=True, last=True, grp={}))
                g = {}
                for kt in range(4 + i, 9 + i):
                    units.append(dict(kind="win", hk=hk, i=i, kt=kt, first=(kt == 4 + i), last=(kt == 8 + i), grp=g))
                g = {}
                for kt in range(0, 9 + i):
                    units.append(dict(kind="sel", hk=hk, i=i, kt=kt, first=(kt == 0), last=(kt == 8 + i), grp=g))

        def u_qk(u):
            hk, i, kt = u["hk"], u["i"], u["kt"]
            pr = Sring.next()
            u["pr"] = pr
            if u["kind"] == "cmp":
                S.mm(qk_fns(pr, kcmpT2, hk, slice(0, 127), 127, i), reads=[kcmpT2.buf, qT.buf], writes=[pr[1].buf, pr[2].buf])
            elif u["kind"] == "win":
                S.mm(qk_fns(pr, kwT2, hk, slice(kt * 128, (kt + 1) * 128), 128, i),
                     reads=[kwT2.buf, qT.buf], writes=[pr[1].buf, pr[2].buf])
            else:
                S.mm(qk_fns(pr, ksT2, hk, slice(kt * 128, (kt + 1) * 128), 128, i, bias_kt=kt),
                     reads=[emt.buf, selT.buf, ksT2.buf, qT.buf], writes=[pr[1].buf, pr[2].buf])

        def u_post(u):
            hk, i, kt, kind = u["hk"], u["i"], u["kt"], u["kind"]
            qsl = slice(i * 128, (i + 1) * 128)
            pr = u["pr"]
            pt = PTring.next()
            kp = 127 if kind == "cmp" else 128
            exp_pair(pr, pt, kp)
            if kind == "cmp":
                mask_mul(pt, 127, cmk[0:127, qsl], cmk.buf)
            elif kind == "win":
                if kt == 4 + i:
                    mask_mul(pt, 128, lmb[:], lmb.buf)
                if kt == 8 + i:
                    mask_mul(pt, 128, dmb[:], dmb.buf)
            else:
                if kt == 8 + i:
                    mask_mul(pt, 128, dmb[:], dmb.buf)
            if u["first"]:
                u["grp"]["acc"] = accring.next()
            acc = u["grp"]["acc"]
            if kind == "cmp":
                pv(acc, pt, 127, vcA[0:127, hk, :], vcA.buf, 97, True, True)
            elif kind == "win":
                pv(acc, pt, 128, vwA[:, kt, hk, :], vwA.buf, 65, u["first"], u["last"])
            else:
                pv(acc, pt, 128, vsA[:, kt, hk, :], vsA.buf, 65, u["first"], u["last"])
            if not u["last"]:
                return
            if kind == "win":
                evac(acc, hk, i, 2)
            elif kind == "sel":
                evac(acc, hk, i, 1)
            else:
                imp = evac(acc, hk, i, 0, want_imp=True)
                sc = A.alloc("sc", [32], F32)
                wk_ = A.alloc("wk", [32], F32)
                m8 = A.alloc("m8", [16], F32)
                sm1 = A.alloc("sm1", [96], BF16)
                S.op("pool", lambda e: e.memset(sm1[:, 32:64], 0.0), writes=[sm1.buf])
                S.op("dve", lambda e: e.tensor_tensor(out=sc[:], in0=imp[:], in1=cstt[:, i, :], op=ALU.add),
                     reads=[imp.buf, cstt.buf], writes=[sc.buf])
                S.op("dve", lambda e: e.max(out=m8[:, 0:8], in_=sc[:]), reads=[sc.buf], writes=[m8.buf])
                S.op("dve", lambda e: e.match_replace(out=wk_[:], in_to_replace=m8[:, 0:8], in_values=sc[:], imm_value=-1e30),
                     reads=[sc.buf, m8.buf], writes=[wk_.buf])
                S.op("dve", lambda e: e.max(out=m8[:, 8:16], in_=wk_[:]), reads=[wk_.buf], writes=[m8.buf])
                S.op("dve", lambda e: e.tensor_scalar(out=m8[:, 0:1], in0=m8[:, 15:16], scalar1=0.0, scalar2=None, op0=ALU.max),
                     reads=[m8.buf], writes=[m8.buf])
                S.op("dve", lambda e: e.tensor_scalar(out=sm1[:, 0:32], in0=sc[:], scalar1=m8[:, 0:1], scalar2=-1.0,
                                                      op0=ALU.is_ge, op1=ALU.add), reads=[sc.buf, m8.buf], writes=[sm1.buf])
                S.op("dve", lambda e: e.tensor_copy(out=sm1[:, 64:96], in_=sm1[:, 0:32]), reads=[sm1.buf], writes=[sm1.buf])
                pm = accring.items[accring.i]
                pmb = bank_bf(pm)
                S.mm([lambda e: e.transpose(pmb[0:96, 0:128], sm1[:], identb[:])], reads=[sm1.buf, identb.buf], writes=[pm.buf])
                S.op("act", lambda e: e.activation(out=selT[0:96, hk, qsl], in_=pmb[0:96, 0:128], func=AF.Copy),
                     reads=[pm.buf], writes=[selT.buf])
                A.free(imp, sc, wk_, m8, sm1)

        DEPTH = 2
        for n in range(min(DEPTH, len(units))):
            u_qk(units[n])
        for n in range(len(units)):
            if n + DEPTH < len(units):
                u_qk(units[n + DEPTH])
            u_post(units[n])
        A.free(cmk, cstt, emt, dmb, lmb, selT, qT, ksT2, kwT2, vsA, vwA, kcmpT2, vcA, gsig, negM, *PTring.items)

        catTn = A.alloc("catTn", [8, TOK], BF16)
        ssn = A.alloc("ssn", [NT], F32)
        for i in range(NT):
            sq = A.alloc("osq", [1024], F32)
            S.op("act", lambda e: e.activation(out=sq[:], in_=onsa[:, i, :], func=AF.Square), reads=[onsa.buf], writes=[sq.buf])
            S.op("dve", lambda e: e.reduce_sum(out=ssn[:, i:i + 1], in_=sq[:], axis=AX.X), reads=[sq.buf], writes=[ssn.buf])
            A.free(sq)
            for c4 in range(2):
                p3 = psring.next()
                S.mm([(lambda e, c=c: e.transpose(p3[:, (c % 4) * 128:(c % 4 + 1) * 128], onsa[:, i, c * 128:(c + 1) * 128], ident[:]))
                      for c in range(4 * c4, 4 * c4 + 4)], reads=[onsa.buf, ident.buf], writes=[p3.buf])
                for c in range(4 * c4, 4 * c4 + 4):
                    S.op("act", lambda e, c=c, p3=p3: e.activation(out=catTn[:, c, i * 128:(i + 1) * 128],
                                                                   in_=p3[:, (c % 4) * 128:(c % 4 + 1) * 128], func=AF.Copy,
                                                                   scale=goutt[:, c:c + 1]),
                         reads=[p3.buf, goutt.buf], writes=[catTn.buf])
        if "nsa" in dbg:
            dbg_dump("onsa", onsa[:], [128, NT, 1024], F32, [onsa.buf])
            dbg_dump("catTn", catTn[:], [128, 8, TOK], BF16, [catTn.buf])
            dbg_dump("ssn", ssn[:], [128, NT], F32, [ssn.buf])
        A.free(onsa)
        if stop == "nsa":
            finish_all()
            return nc, declared, list(dbg_outs.keys())

        A.free(dmt, bspt, svt)
        wo = A.alloc("wo", [DC, D], BF16)
        for nb in range(4):
            S.dma("pool", wo[:, :, nb * 512:(nb + 1) * 512],
                  w_out[:, nb * 512:(nb + 1) * 512].rearrange("(c p) f -> p c f", p=128), writes=[wo.buf])
        stn = A.alloc("stn", [4, NT], F32)
        stg = A.alloc("stg", [4, NT], F32)
        S.op("dve", lambda e: e.tensor_copy(out=stn[:, 0, :], in_=ssn[:]), reads=[ssn.buf], writes=[stn.buf])
        S.op("dve", lambda e: e.tensor_tensor(out=stg[:, 0, :], in0=ssg[:, :, 0], in1=ssg[:, :, 1], op=ALU.add),
             reads=[ssg.buf], writes=[stg.buf])
        rstd_from_ss(stn, NT, 1.0 / 1024)
        rstd_from_ss(stg, NT, 1.0 / 1024)
        h2T = A.alloc("h2T", [DC, TOK], BF16)
        wT = A.alloc("wT", [TOK], F32)
        wrt = A.alloc("wrt", [DC, NE], F32)
        S.dma("sp", wrt[:], w_r.rearrange("(c p) e -> p c e", p=128), writes=[wrt.buf])
        rbb = bc_load("rbb", r_bias, NE)
        ybufs = [Buf("y%d" % i) for i in range(NT)]
        for i in range(NT):
            tsl = slice(i * 128, (i + 1) * 128)
            xt = A.alloc("xt5", [D], F32)
            x1 = A.alloc("x1", [D], F32)
            S.dma("sp", xt[:], xkv[(8 + i) * 128:(9 + i) * 128, :], writes=[xt.buf])
            for nb in range(4):
                nsl = slice(nb * 512, (nb + 1) * 512)
                pa = psring.next()
                S.mm([(lambda e, c=c: e.matmul(pa[:, :], catTn[:, c, tsl], wo[:, c, nsl], start=(c == 0), stop=(c == 7)))
                      for c in range(8)], reads=[catTn.buf, wo.buf], writes=[pa.buf])
                pb_ = psring.next()
                S.mm([(lambda e, c=c: e.matmul(pb_[:, :], catTg[:, c, tsl], wo[:, 8 + c, nsl], start=(c == 0), stop=(c == 7)))
                      for c in range(8)], reads=[catTg.buf, wo.buf], writes=[pb_.buf])
                tmp = A.alloc("mixtmp", [512], F32)
                S.op("dve", lambda e: e.tensor_scalar(out=tmp[:], in0=pa[:, :], scalar1=stn[:, 3, i:i + 1], scalar2=None,
                                                      op0=ALU.mult), reads=[pa.buf, stn.buf], writes=[tmp.buf])
                S.op("dve", lambda e: e.scalar_tensor_tensor(out=tmp[:], in0=pb_[:, :], scalar=stg[:, 3, i:i + 1], in1=tmp[:],
                                                             op0=ALU.mult, op1=ALU.add),
                     reads=[pb_.buf, stg.buf, tmp.buf], writes=[tmp.buf])
                S.op("dve", lambda e: e.tensor_tensor(out=tmp[:], in0=tmp[:], in1=gt1b[:, nsl], op=ALU.mult),
                     reads=[tmp.buf, gt1b.buf], writes=[tmp.buf])
                S.op("dve", lambda e: e.tensor_tensor(out=x1[:, nsl], in0=tmp[:], in1=xt[:, nsl], op=ALU.add),
                     reads=[tmp.buf, xt.buf], writes=[x1.buf])
                A.free(tmp)
            S.dma("sp", y[tsl, :], x1[:], reads=[x1.buf], writes=[ybufs[i]])
            h2f = A.alloc("h2f", [DC, 128], F32)
            norm_transpose(x1[:], x1.buf, gsc2, 48, None, None, 0, f32dst=h2f)
            S.op("pool", lambda e: e.tensor_copy(out=h2T[:, :, tsl], in_=h2f[:]), reads=[h2f.buf], writes=[h2T.buf])
            pl = psring.next()
            S.mm([(lambda e, c=c: e.matmul(pl[:, 0:NE], h2f[:, c, :], wrt[:, c, :], start=(c == 0), stop=(c == DC - 1)))
                  for c in range(DC)], reads=[h2f.buf, wrt.buf], writes=[pl.buf])
            r_s = A.alloc("r_s", [NE], F32)
            r_sb = A.alloc("r_sb", [NE], F32)
            r_g8 = A.alloc("r_g8", [8, 8], F32)
            r_m = A.alloc("r_m", [4, 8], F32)
            r_w = A.alloc("r_w", [NE], F32)
            S.op("act", lambda e: e.activation(out=r_s[:], in_=pl[:, 0:NE], func=AF.Sigmoid), reads=[pl.buf], writes=[r_s.buf])
            S.op("dve", lambda e: e.tensor_tensor(out=r_sb[:], in0=r_s[:], in1=rbb[:], op=ALU.add),
                 reads=[r_s.buf, rbb.buf], writes=[r_sb.buf])
            for g in range(8):
                S.op("dve", lambda e, g=g: e.max(out=r_g8[:, g, :], in_=r_sb[:, g * 8:(g + 1) * 8]),
                     reads=[r_sb.buf], writes=[r_g8.buf])
            S.op("dve", lambda e: e.tensor_tensor(out=r_m[:, 0, :], in0=r_g8[:, :, 0], in1=r_g8[:, :, 1], op=ALU.add),
                 reads=[r_g8.buf], writes=[r_m.buf])
            S.op("dve", lambda e: e.max(out=r_m[:, 1, :], in_=r_m[:, 0, :]), reads=[r_m.buf], writes=[r_m.buf])
            S.op("dve", lambda e: e.tensor_scalar(out=r_m[:, 2, :], in0=r_m[:, 0, :], scalar1=r_m[:, 1, 3:4], scalar2=None,
                                                  op0=ALU.is_ge), reads=[r_m.buf], writes=[r_m.buf])
            S.op("dve", lambda e: e.tensor_scalar(out=r_m[:, 3, :], in0=r_m[:, 2, :], scalar1=4.0, scalar2=-4.0,
                                                  op0=ALU.mult, op1=ALU.add), reads=[r_m.buf], writes=[r_m.buf])
            sb3 = r_sb[:].rearrange("p (g k) -> p g k", k=8)
            S.op("dve", lambda e: e.tensor_tensor(out=sb3, in0=sb3, in1=r_m[:, 2, :].unsqueeze(2).to_broadcast([128, 8, 8]),
                                                  op=ALU.mult), reads=[r_sb.buf, r_m.buf], writes=[r_sb.buf])
            S.op("dve", lambda e: e.tensor_tensor(out=sb3, in0=sb3, in1=r_m[:, 3, :].unsqueeze(2).to_broadcast([128, 8, 8]),
                                                  op=ALU.add), reads=[r_sb.buf, r_m.buf], writes=[r_sb.buf])
            S.op("dve", lambda e: e.max(out=r_m[:, 1, :], in_=r_sb[:]), reads=[r_sb.buf, r_m.buf], writes=[r_m.buf])
            S.op("dve", lambda e: e.tensor_scalar(out=r_w[:], in0=r_sb[:], scalar1=r_m[:, 1, 7:8], scalar2=None, op0=ALU.is_ge),
                 reads=[r_sb.buf, r_m.buf], writes=[r_w.buf])
            S.op("dve", lambda e: e.tensor_tensor(out=r_w[:], in0=r_w[:], in1=r_s[:], op=ALU.mult),
                 reads=[r_w.buf, r_s.buf], writes=[r_w.buf])
            S.op("dve", lambda e: e.reduce_sum(out=r_m[:, 0, 0:1], in_=r_w[:], axis=AX.X), reads=[r_w.buf, r_m.buf], writes=[r_m.buf])
            S.op("dve", lambda e: e.reciprocal(out=r_m[:, 0, 1:2], in_=r_m[:, 0, 0:1]), reads=[r_m.buf], writes=[r_m.buf])
            S.op("dve", lambda e: e.tensor_scalar(out=r_w[:], in0=r_w[:], scalar1=r_m[:, 0, 1:2], scalar2=2.5,
                                                  op0=ALU.mult, op1=ALU.mult), reads=[r_w.buf, r_m.buf], writes=[r_w.buf])
            pt_ = psring.next()
            S.mm([lambda e: e.transpose(pt_[0:NE, 0:128], r_w[:], ident[:])], reads=[r_w.buf, ident.buf], writes=[pt_.buf])
            S.op("act", lambda e: e.activation(out=wT[0:NE, tsl], in_=pt_[0:NE, 0:128], func=AF.Copy),
                 reads=[pt_.buf], writes=[wT.buf])
            A.free(xt, x1, h2f, r_s, r_sb, r_g8, r_m, r_w)
        A.free(wo, catTn, catTg, stn, stg, ssn, ssg, wrt, rbb, gt1b, goutt)
        if "x1" in dbg:
            dbg_dump("wT", wT[0:NE], [NE, TOK], F32, [wT.buf])
            dbg_dump("h2T", h2T[:], [128, DC, TOK], BF16, [h2T.buf])
        if stop == "x1":
            finish_all_y = list(ybufs)
            S.finish("sp", finish_all_y)
            finish_all()
            return nc, declared, list(dbg_outs.keys())

        w_gate = din("w_gate", [NE, D, 512])
        w_up = din("w_up", [NE, D, 512])
        w_down = din("w_down", [NE, 512, D])
        ws_gate = din("ws_gate", [D, 512])
        ws_up = din("ws_up", [D, 512])
        ws_down = din("ws_down", [512, D])
        acc = A.alloc("acc", [NT, D], F32)
        accb = [[Buf("acc%d_%d" % (i, nb)) for nb in range(4)] for i in range(NT)]
        for i in range(NT):
            S.op("pool", lambda e, i=i: e.memset(acc[:, i, :], 0.0), writes=[acc.buf] + accb[i])
        wgu_ring = Ring([A.alloc("wgu%d" % i, [2, DC, 256], BF16) for i in range(2)])
        wd_ring = Ring([A.alloc("wd%d" % i, [4, D], BF16) for i in range(2)])
        hid_ring = Ring([A.alloc("hidT%d" % i, [4, TOK], BF16) for i in range(2)])
        wbs = A.alloc("wbs", [TOK], F32)
        sg_ring = Ring([A.alloc("sg%d" % i, [512], F32) for i in range(2)])
        tt_ring = Ring([A.alloc("tt%d" % i, [512], F32) for i in range(2)])
        gu_ring = Ring(banks[0:4])
        out_ring = Ring(banks[4:7])
        elist = list(range(NE)) + [NE]

        def gu_phase(e_):
            shared = (e_ == NE)
            halves = []
            for fh in range(2):
                wgu = wgu_ring.next()
                gsrc = ws_gate if shared else w_gate[e_]
                usrc = ws_up if shared else w_up[e_]
                S.dma("pool", wgu[:, 0, :, :], gsrc[:, fh * 256:(fh + 1) * 256].rearrange("(c p) f -> p c f", p=128),
                      writes=[wgu.buf])
                S.dma("pool", wgu[:, 1, :, :], usrc[:, fh * 256:(fh + 1) * 256].rearrange("(c p) f -> p c f", p=128),
                      writes=[wgu.buf])
                halves.append(wgu)
            wd_t = wd_ring.next()
            S.dma("pool", wd_t[:], (ws_down if shared else w_down[e_]).rearrange("(c p) d -> p c d", p=128), writes=[wd_t.buf])
            if not shared:
                pw = banks[7]
                for half in range(2):
                    S.mm([lambda e, half=half: e.matmul(pw[:, :], ident[0:NE, e_:e_ + 1].to_broadcast([NE, 128]),
                                                       wT[0:NE, half * 512:(half + 1) * 512], start=True, stop=True)],
                         reads=[ident.buf, wT.buf], writes=[pw.buf])
                    S.op("act", lambda e, half=half: e.activation(out=wbs[:, half * 512:(half + 1) * 512], in_=pw[:, :],
                                                                  func=AF.Copy), reads=[pw.buf], writes=[wbs.buf])
            hidT = hid_ring.next()
            for fh in range(2):
                wgu = halves[fh]
                for fcl in range(2):
                    fc = 2 * fh + fcl
                    for tc in range(2):
                        csl = slice(tc * 512, (tc + 1) * 512)
                        pg = gu_ring.next()
                        S.mm([(lambda e, k=k, pg=pg: e.matmul(pg[:, :], wgu[:, 0, k, fcl * 128:(fcl + 1) * 128], h2T[:, k, csl],
                                                              start=(k == 0), stop=(k == DC - 1))) for k in range(DC)],
                             reads=[wgu.buf, h2T.buf], writes=[pg.buf])
                        pu = gu_ring.next()
                        S.mm([(lambda e, k=k, pu=pu: e.matmul(pu[:, :], wgu[:, 1, k, fcl * 128:(fcl + 1) * 128], h2T[:, k, csl],
                                                              start=(k == 0), stop=(k == DC - 1))) for k in range(DC)],
                             reads=[wgu.buf, h2T.buf], writes=[pu.buf])
                        sg = sg_ring.next()
                        S.op("act", lambda e, sg=sg, pg=pg: e.activation(out=sg[:], in_=pg[:, :], func=AF.Silu),
                             reads=[pg.buf], writes=[sg.buf])
                        if shared:
                            S.op("dve", lambda e, sg=sg, pu=pu, fc=fc, csl=csl: e.tensor_tensor(
                                out=hidT[:, fc, csl], in0=pu[:, :], in1=sg[:], op=ALU.mult),
                                 reads=[pu.buf, sg.buf], writes=[hidT.buf])
                        else:
                            tt = tt_ring.next()
                            S.op("dve", lambda e, sg=sg, pu=pu, tt=tt: e.tensor_tensor(out=tt[:], in0=pu[:, :], in1=sg[:],
                                                                                       op=ALU.mult),
                                 reads=[pu.buf, sg.buf], writes=[tt.buf])
                            S.op("dve", lambda e, tt=tt, fc=fc, csl=csl: e.tensor_tensor(
                                out=hidT[:, fc, csl], in0=tt[:], in1=wbs[:, csl], op=ALU.mult),
                                 reads=[tt.buf, wbs.buf], writes=[hidT.buf])
            return hidT, wd_t

        def down_phase(hidT, wd_t):
            for i in range(NT):
                tsl = slice(i * 128, (i + 1) * 128)
                for nb in range(4):
                    nsl = slice(nb * 512, (nb + 1) * 512)
                    po = out_ring.next()
                    S.mm([(lambda e, fc=fc, po=po: e.matmul(po[:, :], hidT[:, fc, tsl], wd_t[:, fc, nsl],
                                                            start=(fc == 0), stop=(fc == 3))) for fc in range(4)],
                         reads=[hidT.buf, wd_t.buf], writes=[po.buf])
                    S.op("dve", lambda e, po=po, i=i, nsl=nsl: e.tensor_tensor(out=acc[:, i, nsl], in0=acc[:, i, nsl],
                                                                               in1=po[:, :], op=ALU.add),
                         reads=[accb[i][nb], po.buf], writes=[accb[i][nb]])

        prev = gu_phase(elist[0])
        for e_ in elist[1:]:
            cur = gu_phase(e_)
            down_phase(*prev)
            prev = cur
        down_phase(*prev)
        A.free(wbs, h2T, wT, *hid_ring.items, *wgu_ring.items, *wd_ring.items, *sg_ring.items, *tt_ring.items)
        for i in range(NT):
            tsl = slice(i * 128, (i + 1) * 128)
            x1t = A.alloc("x1t", [D], F32)
            S.dma("sp", x1t[:], y[tsl, :], reads=[ybufs[i]], writes=[x1t.buf])
            S.op("dve", lambda e: e.tensor_tensor(out=acc[:, i, :], in0=acc[:, i, :], in1=gt2b[:], op=ALU.mult),
                 reads=accb[i] + [gt2b.buf], writes=accb[i])
            S.op("dve", lambda e: e.tensor_tensor(out=x1t[:], in0=x1t[:], in1=acc[:, i, :], op=ALU.add),
                 reads=accb[i] + [x1t.buf], writes=[x1t.buf])
            S.dma("sp", y[tsl, :], x1t[:], reads=[x1t.buf], writes=[ybufs[i]])
            A.free(x1t)
        S.finish("sp", ybufs)
        finish_all()
        print("program built: ninst=%d sbuf_peak=%d" % (S.ninst, A.peak))
        return nc, declared, list(dbg_outs.keys())


def rope_tables(pos):
    half = 32
    freqs = (10000.0 ** (-np.arange(half, dtype=np.float32) / half)).astype(np.float32)
    ang = pos.astype(np.float32)[:, None] * freqs[None, :]
    return np.cos(ang).astype(np.float32), np.sin(ang).astype(np.float32)


def core_constants(hf):
    cs = {}
    slot = np.arange(SLOTS)
    pos = (slot - 1024 * (1 - hf)).astype(np.float32)
    c, s = rope_tables(pos)
    cs["cos_s"] = np.ascontiguousarray(c.reshape(NS, 128, 32).transpose(1, 0, 2))
    cs["sin_s"] = np.ascontiguousarray(s.reshape(NS, 128, 32).transpose(1, 0, 2))
    n = np.arange(128)
    n_real = n - 64 * (1 - hf)
    c, s = rope_tables(16.0 * n_real + 15.5)
    cs["cos_c"], cs["sin_c"] = c, s
    t_real = hf * 1024 + np.arange(TOK)
    vis = (n_real[:, None] >= 0) & (16 * n_real[:, None] + 31 <= t_real[None, :]) & (n[:, None] < 127)
    cs["cmpmask"] = vis.astype(np.float32)
    j = np.arange(32)
    ov = (16 * n[:, None] <= 64 * j[None, :] + 63) & (16 * n[:, None] + 31 >= 64 * j[None, :]) & (n[:, None] < 127)
    cs["overlap"] = ov.astype(np.float32)
    j_real = j - 16 * (1 - hf)
    cur = t_real // 64
    visible = (j_real[None, :] >= 0) & (j_real[None, :] <= cur[:, None])
    forced = visible & ((j_real[None, :] == 0) | (j_real[None, :] == cur[:, None]) | (j_real[None, :] == cur[:, None] - 1))
    cstv = np.where(visible, np.where(forced, 100.0, 0.0), -1.0).astype(np.float32)
    cs["cst"] = np.ascontiguousarray(cstv.reshape(NT, 128, 32).transpose(1, 0, 2))
    em = np.zeros((32, NS, 128), np.float32)
    for kt in range(NS):
        for p in range(128):
            em[2 * kt + p // 64, kt, p] = BIG
    cs["emat"] = em
    p = np.arange(128)
    cs["dmask"] = (p[:, None] <= p[None, :]).astype(np.float32)
    cs["lmask"] = (p[:, None] > p[None, :]).astype(np.float32)
    sv = (pos >= 0).astype(np.float32)
    cs["svalid"] = np.ascontiguousarray(sv.reshape(NS, 128).T)
    return cs


def prep_inputs(inp):
    f = lambda a: np.ascontiguousarray(np.asarray(a, dtype=np.float32))
    x = f(inp["x"])
    c = f(inp["c"])
    shared = {
        "w_ada": f(inp["w_ada"][0]), "b_ada": f(inp["b_ada"][0][None]),
        "gn1": f(np.asarray(inp["g_norm1"][0]).reshape(DC, 128).T),
        "gn2": f(np.asarray(inp["g_norm2"][0]).reshape(DC, 128).T),
        "gout": f(np.concatenate([np.asarray(inp["g_out_nsa"][0]), np.asarray(inp["g_out_gmlp"][0])]).reshape(DC, 128).T),
        "w_in": f(inp["w_in"][0]),
        "g_q": f(inp["g_q"][0][None]), "g_k": f(inp["g_k"][0][None]),
        "posk": f(np.asarray(inp["cmp_pos_k"][0]).T), "posv": f(np.asarray(inp["cmp_pos_v"][0]).T),
        "w1k": f(inp["cmp_w1_k"][0]), "w1v": f(inp["cmp_w1_v"][0]),
        "w2k": f(inp["cmp_w2_k"][0]), "w2v": f(inp["cmp_w2_v"][0]),
        "g_gv": f(np.asarray(inp["g_gmlp_v"][0]).reshape(1, 1024)),
        "wspT": f(np.asarray(inp["w_spatial"][0]).transpose(0, 2, 1)),
        "bsp": f(np.asarray(inp["b_spatial"][0]).T),
        "w_out": f(inp["w_out"][0]), "w_r": f(inp["w_router"][0]), "r_bias": f(inp["router_bias"][0][None]),
        "w_gate": f(inp["w_gate"][0]), "w_up": f(inp["w_up"][0]), "w_down": f(inp["w_down"][0]),
        "ws_gate": f(inp["ws_gate"][0]), "ws_up": f(inp["ws_up"][0]), "ws_down": f(inp["ws_down"][0]),
    }
    consts = [core_constants(0), core_constants(1)]
    maps = []
    for core in range(8):
        b, hf = core // 2, core % 2
        m = dict(shared)
        if hf == 1:
            m["xkv"] = f(x[b])
        else:
            m["xkv"] = f(np.concatenate([np.zeros((TOK, D), np.float32), x[b, :TOK]], axis=0))
        m["ct"] = f(c[b].reshape(DC, 128).T)
        m.update(consts[hf])
        maps.append(m)
    return maps


def kernel(**inputs):
    nc, declared, _ = build_program()
    maps = prep_inputs(inputs)
    in_maps = [{k: m[k] for k in declared} for m in maps]
    res = run_bass_kernel_spmd(nc, in_maps, core_ids=list(range(8)))
    out = np.zeros((4, 2048, D), np.float32)
    for core in range(8):
        b, hf = core // 2, core % 2
        out[b, hf * TOK:(hf + 1) * TOK] = res.results[core]["y"]
    return out
```
